# Optimizing a Trainium2 kernel written in Bass

```python
import jax
import jax.numpy as jnp
from jax import lax
import numpy as np

D_MODEL = 1024
BATCH = 4
SEQ = 8192
DEPTH = 4

HG_HEADS = 4
HG_DK = 128
HG_DV = 128
HG_CHUNK = 16
LOG_TINY = -87.0
CV_W = 512
CV_K = 31
SB_HEADS = 8
SB_DH = 64
SB_W = SB_HEADS * SB_DH
SB_BLOCK = 128
RT_HEADS = 4
RT_DH = 128
RT_W = RT_HEADS * RT_DH
RT_CHUNK = 128
BRANCH_W = (HG_HEADS * HG_DV, CV_W, SB_W, RT_W)
N_BRANCH = 4
MIX_W = HG_HEADS * HG_DV + CV_W + SB_W + RT_W
IN_SPLIT = (HG_HEADS * HG_DK, HG_HEADS * HG_DK, HG_HEADS * HG_DV, HG_HEADS * HG_DV, 2 * CV_W,
            SB_W, SB_W, SB_W, RT_W, RT_W, RT_W, RT_W)
IN_COLS = 4 * HG_HEADS * HG_DK + 2 * CV_W + 3 * SB_W + 4 * RT_W
N_GROUPS = 4
EXP_PER_GROUP = 8
N_EXPERTS = N_GROUPS * EXP_PER_GROUP
TOP_K = 2
D_EXPERT = 512
MOE_BLOCK = 128
EPS = 1e-6

kernel_name = 'hybrid_gated_hgrn2_conv_stickbreak_retention_hmoe'


def head_rms(t, g):
    tf = t.astype(jnp.float32)
    return tf * lax.rsqrt(jnp.mean(tf * tf, axis=-1, keepdims=True) + EPS) * g.astype(jnp.float32)


def rms_norm(x, g):
    return head_rms(x, g).astype(x.dtype)


def layer_norm(x, g, b):
    xf = x.astype(jnp.float32)
    xc = xf - jnp.mean(xf, axis=-1, keepdims=True)
    return xc * lax.rsqrt(jnp.mean(xc * xc, axis=-1, keepdims=True) + EPS) * g.astype(jnp.float32) + b.astype(jnp.float32)


def hgrn2_mixer(q, f_pre, i_in, g_out, lb, norm_g):
    Bn, S, _ = q.shape
    N, C = S // HG_CHUNK, HG_CHUNK

    def heads(t, d):
        return t.astype(jnp.float32).reshape(Bn, N, C, HG_HEADS, d).transpose(1, 0, 3, 2, 4)

    qh = jax.nn.silu(heads(q, HG_DK))
    fx = heads(f_pre, HG_DK)
    vh = heads(i_in, HG_DV)
    lb_h = lb.astype(jnp.float32).reshape(HG_HEADS, 1, HG_DK)
    log_lb = jnp.maximum(jnp.log(jnp.maximum(lb_h, 1e-30)), LOG_TINY)
    log_f = jnp.logaddexp(log_lb, jnp.log1p(-lb_h) + jax.nn.log_sigmoid(fx))
    kh = (1.0 - lb_h) * jax.nn.sigmoid(-fx)
    b = jnp.cumsum(log_f, axis=3)
    b_last = b[:, :, :, -1:, :]
    causal = jnp.tril(jnp.ones((C, C), dtype=bool))[:, :, None]
    diff = b[:, :, :, :, None, :] - b[:, :, :, None, :, :]
    decay = jnp.where(causal, jnp.exp(jnp.where(causal, diff, 0.0)), 0.0)
    scores = jnp.einsum('nbhtd,nbhsd,nbhtsd->nbhts', qh, kh, decay)
    intra = jnp.einsum('nbhts,nbhsv->nbhtv', scores, vh)
    q_dec = qh * jnp.exp(b)
    k_state = kh * jnp.exp(b_last - b)
    chunk_decay = jnp.exp(b_last[:, :, :, 0, :])

    def step(state, xs):
        q_c, k_c, v_c, dec = xs
        o = jnp.einsum('bhtd,bhdv->bhtv', q_c, state)
        state = dec[..., None] * state + jnp.einsum('bhsd,bhsv->bhdv', k_c, v_c)
        return state, o

    s0 = jnp.zeros((Bn, HG_HEADS, HG_DK, HG_DV), jnp.float32)
    _, inter = lax.scan(step, s0, (q_dec, k_state, vh, chunk_decay))
    o = (intra + inter).transpose(1, 0, 3, 2, 4).reshape(Bn, S, HG_HEADS, HG_DV)
    o = head_rms(o, norm_g.reshape(HG_HEADS, HG_DV)).reshape(Bn, S, HG_HEADS * HG_DV)
    return o * jax.nn.silu(g_out.astype(jnp.float32))


def conformer_conv(u, conv_w, conv_b, ln_g, ln_b):
    a, gate = jnp.split(u, 2, axis=-1)
    z = a * jax.nn.sigmoid(gate)
    z = lax.conv_general_dilated(z, conv_w[:, None, :].astype(z.dtype), window_strides=(1,),
                                 padding=((CV_K - 1, 0),), dimension_numbers=('NWC', 'WIO', 'NWC'),
                                 feature_group_count=CV_W)
    z = z + conv_b
    return jax.nn.silu(layer_norm(z, ln_g, ln_b))


def stick_breaking_attention(q, k, v, qn_g, kn_g):
    Bn, S, _ = q.shape
    nb = S // SB_BLOCK

    def heads(t):
        return t.reshape(Bn, S, SB_HEADS, SB_DH)

    qh = (head_rms(heads(q), qn_g) * (SB_DH ** -0.5)).transpose(0, 2, 1, 3)
    kh = head_rms(heads(k), kn_g).transpose(0, 2, 1, 3)
    vh = heads(v).astype(jnp.float32).transpose(0, 2, 1, 3)
    q_blocks = qh.reshape(Bn, SB_HEADS, nb, SB_BLOCK, SB_DH).transpose(2, 0, 1, 3, 4)
    key_pos = jnp.arange(S)

    def block(args):
        q_b, start = args
        z = jnp.einsum('bhtd,bhsd->bhts', q_b, kh)
        past = key_pos[None, :] < (start + jnp.arange(SB_BLOCK))[:, None]
        log_keep = jnp.where(past, jax.nn.log_sigmoid(-z), 0.0)
        log_between = lax.cumsum(log_keep, axis=3, reverse=True) - log_keep
        w = jnp.where(past, jnp.exp(jax.nn.log_sigmoid(z) + log_between), 0.0)
        return jnp.einsum('bhts,bhsd->bhtd', w, vh)

    o = lax.map(block, (q_blocks, jnp.arange(nb) * SB_BLOCK))
    return o.transpose(1, 0, 3, 2, 4).reshape(Bn, S, SB_W)


def retention(q, k, v, g, norm_g):
    Bn, S, _ = q.shape
    N, C = S // RT_CHUNK, RT_CHUNK

    def heads(t):
        return t.astype(jnp.float32).reshape(Bn, N, C, RT_HEADS, RT_DH).transpose(1, 0, 3, 2, 4)

    qh, kh, vh = heads(q), heads(k) * (RT_DH ** -0.5), heads(v)
    log_gamma = jnp.log1p(-jnp.exp2(-5.0 - jnp.arange(RT_HEADS, dtype=jnp.float32)))[:, None, None]
    pos = jnp.arange(C, dtype=jnp.float32)[:, None]
    rel = pos - pos.T
    decay_mat = jnp.where(rel >= 0, jnp.exp(log_gamma * jnp.maximum(rel, 0.0)), 0.0)
    intra = jnp.einsum('nbhts,nbhsv->nbhtv', jnp.einsum('nbhtd,nbhsd->nbhts', qh, kh) * decay_mat, vh)
    q_dec = qh * jnp.exp(log_gamma * (pos + 1.0))
    k_dec = kh * jnp.exp(log_gamma * (C - 1.0 - pos))
    chunk_decay = jnp.exp(log_gamma * C)

    def step(state, xs):
        q_c, k_c, v_c = xs
        o = jnp.einsum('bhtd,bhdv->bhtv', q_c, state)
        state = chunk_decay * state + jnp.einsum('bhsd,bhsv->bhdv', k_c, v_c)
        return state, o

    s0 = jnp.zeros((Bn, RT_HEADS, RT_DH, RT_DH), jnp.float32)
    _, inter = lax.scan(step, s0, (q_dec, k_dec, vh))
    o = (intra + inter).transpose(1, 0, 3, 2, 4).reshape(Bn, S, RT_HEADS, RT_DH)
    o = head_rms(o, norm_g.reshape(RT_HEADS, RT_DH)).reshape(Bn, S, RT_W)
    return o * jax.nn.silu(g.astype(jnp.float32))


def hybrid_mixer(h, w_in, lb, hg_g, conv_w, conv_b, ln_g, ln_b, qn_g, kn_g, rt_g, w_branch, w_gate, b_gate, w_out):
    offs = [0]
    for w in IN_SPLIT:
        offs.append(offs[-1] + w)

    def proj(j):
        return h @ w_in[:, offs[j]:offs[j + 1]]

    o_hg = hgrn2_mixer(proj(0), proj(1), proj(2), proj(3), lb, hg_g)
    o_cv = conformer_conv(proj(4), conv_w, conv_b, ln_g, ln_b)
    o_sb = stick_breaking_attention(proj(5), proj(6), proj(7), qn_g, kn_g)
    o_rt = retention(proj(8), proj(9), proj(10), proj(11), rt_g)
    merged = jnp.zeros_like(h)
    r = 0
    for n, o in enumerate((o_hg, o_cv, o_sb, o_rt)):
        bw = BRANCH_W[n]
        y = o.astype(h.dtype) @ w_branch[r:r + bw]
        r += bw
        gate = jax.nn.sigmoid(h @ w_gate[:, n * D_MODEL:(n + 1) * D_MODEL] + b_gate[n * D_MODEL:(n + 1) * D_MODEL])
        merged = merged + gate * y
    return merged @ w_out


def hier_moe(h, wg, bg, we, be, w1, w3, w2):
    Bn, S, D = h.shape
    T = Bn * S
    hf = h.reshape(T, D)
    g_prob = jax.nn.softmax((hf @ wg + bg).astype(jnp.float32), axis=-1)
    g_p, g_idx = lax.top_k(g_prob, 1)
    e_logits = (hf @ we + be).astype(jnp.float32).reshape(T, N_GROUPS, EXP_PER_GROUP)
    e_in_group = jnp.take_along_axis(e_logits, g_idx[:, :, None], axis=1)[:, 0]
    top_v, top_i = lax.top_k(e_in_group, TOP_K)
    gate = g_p * jax.nn.softmax(top_v, axis=-1)
    expert = g_idx * EXP_PER_GROUP + top_i
    A = T * TOP_K
    flat_e = expert.reshape(A)
    flat_t = jnp.repeat(jnp.arange(T, dtype=jnp.int32), TOP_K)
    flat_g = gate.reshape(A)
    order = jnp.argsort(flat_e)
    e_sorted = flat_e[order]
    counts = jnp.bincount(flat_e, length=N_EXPERTS)
    padded = (counts + MOE_BLOCK - 1) // MOE_BLOCK * MOE_BLOCK
    pad_end = jnp.cumsum(padded)
    pad_start = pad_end - padded
    grp_start = jnp.cumsum(counts) - counts
    dest = pad_start[e_sorted] + jnp.arange(A, dtype=jnp.int32) - grp_start[e_sorted]
    P = (A + N_EXPERTS * (MOE_BLOCK - 1) + MOE_BLOCK - 1) // MOE_BLOCK * MOE_BLOCK
    NB = P // MOE_BLOCK
    row_tok = jnp.full((P,), T, jnp.int32).at[dest].set(flat_t[order])
    row_gate = jnp.zeros((P,), jnp.float32).at[dest].set(flat_g[order])
    blk_expert = jnp.minimum(jnp.searchsorted(pad_end, jnp.arange(NB) * MOE_BLOCK, side='right'), N_EXPERTS - 1)
    x_pad = jnp.concatenate([hf, jnp.zeros((1, D), hf.dtype)], axis=0)
    xb = x_pad[row_tok].reshape(NB, MOE_BLOCK, D)

    def expert_block(args):
        xe, e = args
        return (jax.nn.silu(xe @ w1[e]) * (xe @ w3[e])) @ w2[e]

    yb = lax.map(expert_block, (xb, blk_expert)).reshape(P, D)
    yb = yb * row_gate[:, None].astype(yb.dtype)
    out = jax.ops.segment_sum(yb, row_tok, num_segments=T + 1)[:T]
    return out.reshape(Bn, S, D)


def setup_inputs(seed: int = 0) -> dict:
    key = jax.random.key(seed)
    ks = jax.random.split(key, 27)
    n = lambda k, s, sc: jax.random.normal(k, s, jnp.float32) * sc
    L, D = DEPTH, D_MODEL
    return {
        'x': n(ks[0], (BATCH, SEQ, D), 1.0),
        'c': n(ks[1], (BATCH, D), 1.0),
        'ada_w': n(ks[2], (L, D, 6 * D), 0.5 * D ** -0.5),
        'ada_b': n(ks[3], (L, 6 * D), 0.01),
        'norm1_g': 1.0 + n(ks[4], (L, D), 0.02),
        'norm2_g': 1.0 + n(ks[5], (L, D), 0.02),
        'w_in': n(ks[6], (L, D, IN_COLS), D ** -0.5),
        'hgrn_lb': n(ks[7], (L, HG_HEADS * HG_DK), 0.1),
        'hgrn_norm_g': 1.0 + n(ks[8], (L, HG_HEADS * HG_DV), 0.02),
        'conv_w': n(ks[9], (L, CV_K, CV_W), CV_K ** -0.5),
        'conv_b': n(ks[10], (L, CV_W), 0.01),
        'conv_ln_g': 1.0 + n(ks[11], (L, CV_W), 0.02),
        'conv_ln_b': n(ks[12], (L, CV_W), 0.01),
        'sb_qnorm_g': 1.0 + n(ks[13], (L, SB_DH), 0.02),
        'sb_knorm_g': 1.0 + n(ks[14], (L, SB_DH), 0.02),
        'ret_norm_g': 1.0 + n(ks[15], (L, RT_W), 0.02),
        'w_branch': n(ks[16], (L, MIX_W, D), 512 ** -0.5),
        'w_gate': n(ks[17], (L, D, N_BRANCH * D), D ** -0.5),
        'b_gate': n(ks[18], (L, N_BRANCH * D), 0.01),
        'w_out': n(ks[19], (L, D, D), D ** -0.5),
        'router_group_w': n(ks[20], (L, D, N_GROUPS), D ** -0.5),
        'router_group_b': n(ks[21], (L, N_GROUPS), 0.01),
        'router_expert_w': n(ks[22], (L, D, N_EXPERTS), D ** -0.5),
        'router_expert_b': n(ks[23], (L, N_EXPERTS), 0.01),
        'expert_w1': n(ks[24], (L, N_EXPERTS, D, D_EXPERT), D ** -0.5),
        'expert_w3': n(ks[25], (L, N_EXPERTS, D, D_EXPERT), D ** -0.5),
        'expert_w2': n(ks[26], (L, N_EXPERTS, D_EXPERT, D), D_EXPERT ** -0.5),
    }


def reference(x, c, ada_w, ada_b, norm1_g, norm2_g, w_in, hgrn_lb, hgrn_norm_g, conv_w, conv_b, conv_ln_g,
              conv_ln_b, sb_qnorm_g, sb_knorm_g, ret_norm_g, w_branch, w_gate, b_gate, w_out, router_group_w,
              router_group_b, router_expert_w, router_expert_b, expert_w1, expert_w3, expert_w2):
    sm = jax.nn.softmax(hgrn_lb.astype(jnp.float32), axis=0)
    lower_bounds = jnp.cumsum(sm, axis=0) - sm[0]
    mods = jnp.einsum('bd,lde->lbe', jax.nn.silu(c), ada_w) + ada_b[:, None, :]
    for l in range(DEPTH):
        sh1, sc1, g1, sh2, sc2, g2 = jnp.split(mods[l][:, None, :], 6, axis=-1)
        h = rms_norm(x, norm1_g[l]) * (1.0 + sc1) + sh1
        x = x + g1 * hybrid_mixer(h, w_in[l], lower_bounds[l], hgrn_norm_g[l], conv_w[l], conv_b[l], conv_ln_g[l],
                                  conv_ln_b[l], sb_qnorm_g[l], sb_knorm_g[l], ret_norm_g[l], w_branch[l], w_gate[l],
                                  b_gate[l], w_out[l])
        h = rms_norm(x, norm2_g[l]) * (1.0 + sc2) + sh2
        x = x + g2 * hier_moe(h, router_group_w[l], router_group_b[l], router_expert_w[l], router_expert_b[l],
                              expert_w1[l], expert_w3[l], expert_w2[l])
    return x
```

```python
import numpy as np
from contextlib import ExitStack, contextmanager
import concourse.bass as bass
import concourse.mybir as mybir
from concourse.bass_utils import run_bass_kernel_spmd

F32 = mybir.dt.float32
BF16 = mybir.dt.bfloat16
AF = mybir.ActivationFunctionType
ALU = mybir.AluOpType
AX = mybir.AxisListType

D = 1024
KD = 8
IN_COLS = 6656
EPS = 1e-6
N_EXP = 32
HG_CHK = 32
D_EXP = 512
SAME_ENG_SYNC = True
SAME_ENG_MIN_FREE = 512
FRESH_SWDGE_SEM = False

VOFF = {}
_o = 0
for _n, _w in (("n1g", 8), ("n2g", 8), ("adab", 48), ("hgng", 4), ("cvb", 4), ("lng", 4), ("lnb", 4), ("qng", 1),
               ("kng", 1), ("rtg", 4), ("bgate", 32), ("cvw", 124), ("hlb", 4)):
    VOFF[_n] = _o
    _o += _w
NV = _o


class DSem:
    def __init__(self, sem, cnt=0):
        self.sem = sem
        self.cnt = cnt


class Buf:
    def __init__(self, t, name):
        self.t = t
        self.name = name
        n = 1
        for d_ in list(t.shape)[1:]:
            n *= int(d_)
        self.small = n < SAME_ENG_MIN_FREE
        self.last_w = None
        self.reads = {}
        self.dsem = None

    def __getitem__(self, idx):
        return self.t[idx]


class Prog:
    ENGS = ("pe", "act", "dve", "pool", "sp")

    def __init__(self, nc):
        self.nc = nc
        self.root = ExitStack()
        self.stacks = [self.root]
        self.eng = {"pe": nc.tensor, "act": nc.scalar, "dve": nc.vector, "pool": nc.gpsimd, "sp": nc.sync}
        self.q = {e: [] for e in self.ENGS}
        self.cnt = {e: 0 for e in self.ENGS}
        self.sem = {e: self.root.enter_context(nc.semaphore("s_" + e)) for e in self.ENGS}
        self.known = {e: {} for e in self.ENGS}
        self.nbuf = 0
        self.sempool = []
        self.scope_bufs = [[]]
        self.live_dsems = []
        self.ninstr = 0

    @contextmanager
    def scope(self):
        st = ExitStack()
        self.stacks.append(st)
        self.scope_bufs.append([])
        try:
            yield
        finally:
            self.barrier()
            for b in self.scope_bufs.pop():
                if b.dsem is not None:
                    self.sempool.append(b.dsem)
                    b.dsem = None
            self.stacks.pop()
            st.close()

    def _reg(self, t, name):
        b = Buf(t, name)
        self.scope_bufs[-1].append(b)
        return b

    def sb(self, shape, dt, name=None):
        self.nbuf += 1
        name = f"{name or 'sb'}_{self.nbuf}"
        return self._reg(self.stacks[-1].enter_context(self.nc.sbuf_tensor(name, list(shape), dt)), name)

    def ps(self, shape, dt, name=None):
        self.nbuf += 1
        name = f"{name or 'ps'}_{self.nbuf}"
        return self._reg(self.stacks[-1].enter_context(self.nc.psum_tensor(name, list(shape), dt)), name)

    def dram(self, name, shape, dt, kind="Internal"):
        return self.nc.dram_tensor(name, list(shape), dt, kind=kind).ap()

    def _wait(self, e, key, val, small=True):
        if key == e and (e == "pe" or not SAME_ENG_SYNC or not small):
            return
        if self.known[e].get(key, 0) >= val:
            return
        self.known[e][key] = val
        sem = self.sem[key] if isinstance(key, str) else key.sem
        self.q[e].append(lambda en, sem=sem, val=val: en.wait_ge(sem, val))
        self.ninstr += 1

    def _deps(self, e, reads, writes):
        for b in reads:
            if b.last_w is not None:
                self._wait(e, *b.last_w, small=b.small)
        for b in writes:
            if b.last_w is not None:
                self._wait(e, *b.last_w, small=b.small)
            for k, v in b.reads.items():
                self._wait(e, k, v, small=b.small)

    def _commit(self, ev, reads, writes):
        for b in reads:
            if b.reads.get(ev[0], 0) < ev[1]:
                b.reads[ev[0]] = ev[1]
        for b in writes:
            b.last_w = ev
            b.reads = {}

    def op(self, e, fn, reads=(), writes=()):
        self._deps(e, reads, writes)
        self.cnt[e] += 1
        ev = (e, self.cnt[e])
        sem = self.sem[e]
        self.q[e].append(lambda en, fn=fn, sem=sem: fn(en).then_inc(sem, 1))
        self.ninstr += 1
        self._commit(ev, reads, writes)

    def dma(self, e, out_ap, in_ap, reads=(), writes=(), owner=None, **kw):
        self._deps(e, reads, writes)
        if FRESH_SWDGE_SEM and e == "pool":
            owner.dsem = DSem(self.root.enter_context(self.nc.semaphore(f"f{len(self.live_dsems)}")))
            self.live_dsems.append(owner.dsem)
        if owner.dsem is None:
            if self.sempool:
                owner.dsem = self.sempool.pop()
            else:
                owner.dsem = DSem(self.root.enter_context(self.nc.semaphore(f"d{len(self.live_dsems)}")))
                self.live_dsems.append(owner.dsem)
        ds = owner.dsem
        ds.cnt += 16
        ev = (ds, ds.cnt)
        self.q[e].append(lambda en, o=out_ap, i=in_ap, sem=ds.sem, kw=kw: en.dma_start(out=o, in_=i, **kw).then_inc(sem, 16))
        self.ninstr += 1
        self._commit(ev, reads, writes)

    def barrier(self):
        evs = [(k, self.cnt[k]) for k in self.ENGS if self.cnt[k] > 0]
        evs += [(ds, ds.cnt) for ds in self.live_dsems if ds.cnt > 0]
        for e in self.ENGS:
            for k, v in evs:
                if k == e:
                    if e != "pe" and self.known[e].get(e, 0) < v:
                        self.known[e][e] = v
                        self.q[e].append(lambda en, sem=self.sem[e], val=v: en.wait_ge(sem, val))
                    continue
                self._wait(e, k, v)

    def emit(self):
        nc = self.nc
        with nc.Block() as block:
            @block.tensor
            def _(en):
                for f in self.q["pe"]:
                    f(en)

            @block.scalar
            def _(en):
                for f in self.q["act"]:
                    f(en)

            @block.vector
            def _(en):
                for f in self.q["dve"]:
                    f(en)

            @block.gpsimd
            def _(en):
                for f in self.q["pool"]:
                    f(en)

            @block.sync
            def _(en):
                for f in self.q["sp"]:
                    f(en)
        self.root.close()


def host_consts():
    import ml_dtypes
    bf = ml_dtypes.bfloat16
    c = {}
    c["ident_f"] = np.eye(128, dtype=np.float32)
    c["ident_b"] = np.eye(128).astype(bf)
    c["ones_f"] = np.ones((128, 128), np.float32)
    blk = np.zeros((128, 128), np.float32)
    blk[:64, :64] = 1
    blk[64:, 64:] = 1
    c["blkones_f"] = blk
    s = np.arange(128)[:, None].astype(np.float64)
    t = np.arange(128)[None, :].astype(np.float64)
    dec = np.zeros((128, 4, 512), np.float64)
    gp = np.zeros((128, 4, 512), np.float64)
    kd = np.zeros((128, 512), np.float64)
    for h in range(4):
        lg = np.log1p(-(2.0 ** (-5.0 - h)))
        m = np.where(t >= s, np.exp(lg * np.maximum(t - s, 0)), 0.0) * (128 ** -0.5)
        dec[:, h, :] = np.tile(m, (1, 4))
        gp[:, h, :] = np.tile(np.exp(lg * (np.arange(128) + 1.0))[None, :], (128, 4))
        kd[:, h * 128:(h + 1) * 128] = (np.exp(lg * (127.0 - np.arange(128))) * (128 ** -0.5))[:, None]
    c["ret_dec"] = dec.astype(np.float32)
    c["ret_gp"] = gp.astype(np.float32)
    c["ret_kd"] = kd.astype(np.float32)
    same = (np.arange(128)[:, None] // HG_CHK) == (np.arange(128)[None, :] // HG_CHK)
    m = (same & (np.arange(128)[:, None] <= np.arange(128)[None, :])).astype(np.float32)
    c["hg_mask"] = np.tile(m, (1, 4)).astype(np.float32)
    sm = np.ones((128, 2048), np.float32)
    sm[:, ::HG_CHK] = 0.0
    c["hg_scanmask"] = sm
    c["sb_trineg"] = (-(np.arange(128)[:, None] >= np.arange(128)[None, :]).astype(np.float32)).astype(bf)
    c["sb_onesneg"] = (-np.ones((128, 128), np.float32)).astype(bf)
    dm = np.zeros((128, 8, 1024), np.float32)
    for j in range(8):
        dm[:, j, :] = ((128 * j + np.arange(128))[:, None] < np.arange(1024)[None, :]).astype(np.float32)
    c["sb_dmask"] = dm.astype(bf)
    c["moe_slt"] = (np.arange(128)[:, None] < np.arange(128)[None, :]).astype(np.float32).astype(bf)
    c["ones_b"] = np.ones((128, 128), np.float32).astype(bf)
    c["moe_iota"] = np.ascontiguousarray(np.broadcast_to((np.arange(64) - 1000.0).astype(np.float32)[None, :], (128, 64)))
    return c


CONST_SPECS = {
    "ident_f": ([128, 128], F32), "ident_b": ([128, 128], BF16), "ones_f": ([128, 128], F32),
    "blkones_f": ([128, 128], F32), "ret_dec": ([128, 4, 512], F32), "ret_gp": ([128, 4, 512], F32),
    "ret_kd": ([128, 512], F32), "hg_mask": ([128, 512], F32), "hg_scanmask": ([128, 2048], F32),
    "moe_slt": ([128, 128], BF16), "ones_b": ([128, 128], BF16), "moe_iota": ([128, 64], F32),
    "sb_trineg": ([128, 128], BF16), "sb_onesneg": ([128, 128], BF16), "sb_dmask": ([128, 8, 1024], BF16),
}


class Cfg:
    def __init__(self, S=8192, L=4, debug=()):
        self.S = S
        self.L = L
        self.NTB = S // 512
        self.NT = S // 128
        self.debug = set(debug)


def build_program(cfg):
    nc = bass.Bass("TRN2", target_bir_lowering=False)
    P = Prog(nc)
    S, L = cfg.S, cfg.L
    dbg = cfg.debug

    def OP(e, fn, r=(), w=()):
        P.op(e, fn, reads=r, writes=w)

    def MM(out, lhsT, rhs, start=True, stop=True, r=(), w=()):
        P.op("pe", lambda en: en.matmul(out, lhsT, rhs, start=start, stop=stop), reads=r, writes=w)

    def TR(out, in_, ident, r=(), w=()):
        P.op("pe", lambda en: en.transpose(out, in_, ident), reads=r, writes=w)

    def ACT(out, in_, func, bias=None, scale=None, r=(), w=(), accum=None):
        kw = {}
        if bias is not None:
            kw["bias"] = bias
        if scale is not None:
            kw["scale"] = scale
        if accum is not None:
            kw["accum_out"] = accum
        P.op("act", lambda en: en.activation(out, in_, func, **kw), reads=r, writes=w)

    def TT(e, out, in0, in1, op, r=(), w=()):
        P.op(e, lambda en: en.tensor_tensor(out, in0, in1, op), reads=r, writes=w)

    def TS(e, out, in0, s1, s2, op0, op1=None, r=(), w=()):
        if op1 is None:
            P.op(e, lambda en: en.tensor_scalar(out, in0, s1, None, op0), reads=r, writes=w)
        else:
            P.op(e, lambda en: en.tensor_scalar(out, in0, s1, s2, op0, op1), reads=r, writes=w)

    def STT(out, in0, scalar, in1, op0, op1, r=(), w=()):
        P.op("dve", lambda en: en.scalar_tensor_tensor(out, in0, scalar, in1, op0, op1), reads=r, writes=w)

    def CP(e, out, in_, r=(), w=()):
        if e == "act":
            P.op("act", lambda en: en.activation(out, in_, AF.Identity), reads=r, writes=w)
        else:
            P.op(e, lambda en: en.tensor_copy(out, in_), reads=r, writes=w)

    def LD(out_buf, out_ap, in_ap, e="sp"):
        P.dma(e, out_ap, in_ap, writes=[out_buf], owner=out_buf)

    def ST(out_ap, in_buf, in_ap, e="pool"):
        P.dma(e, out_ap, in_ap, reads=[in_buf], owner=in_buf)

    xT_in = P.dram("xT", [D, S], F32, kind="ExternalInput")
    c_in = P.dram("c", [128, KD], F32, kind="ExternalInput")
    vecs_in = P.dram("vecs", [L, 128, NV], F32, kind="ExternalInput")
    ada_w = P.dram("ada_w", [L, D, 6 * D], F32, kind="ExternalInput")
    w_in = P.dram("w_in", [L, D, IN_COLS], F32, kind="ExternalInput")
    w_branch = P.dram("w_branch", [L, 2048, D], F32, kind="ExternalInput")
    w_gate = P.dram("w_gate", [L, D, 4 * D], F32, kind="ExternalInput")
    w_out = P.dram("w_out", [L, D, D], F32, kind="ExternalInput")
    rw_in = P.dram("rw", [L, D, 36], F32, kind="ExternalInput")
    rb_in = P.dram("rb", [L, 128, 36], F32, kind="ExternalInput")
    ew1 = P.dram("ew1", [L, N_EXP, D, D_EXP], F32, kind="ExternalInput")
    ew3 = P.dram("ew3", [L, N_EXP, D, D_EXP], F32, kind="ExternalInput")
    ew2 = P.dram("ew2", [L, N_EXP, D_EXP, D], F32, kind="ExternalInput")
    cin = {k: P.dram("k_" + k, shp, dt, kind="ExternalInput") for k, (shp, dt) in CONST_SPECS.items()}
    out_T = P.dram("outT", [D, S], F32, kind="ExternalOutput")

    def scratch(name, shape, dt):
        return P.dram(name, shape, dt, kind=("ExternalOutput" if name in dbg else "Internal"))

    xA = scratch("xA", [D, S], F32)
    xB = scratch("xB", [D, S], F32)
    hT_d = scratch("hT", [D, S], BF16)
    hq_d = scratch("hq", [512, S], BF16)
    hlf_d = scratch("hlf", [512, S], F32)
    hk_d = scratch("hk", [512, S], BF16)
    hg_d = scratch("hg", [512, S], BF16)
    hv_d = scratch("hv", [S, 512], BF16)
    cz_d = scratch("cz", [512, S], BF16)
    sq_d = scratch("sq", [512, S], BF16)
    sk_d = scratch("sk", [512, S], BF16)
    sv_d = scratch("sv", [S, 512], BF16)
    rq_d = scratch("rq", [512, S], BF16)
    rk_d = scratch("rk", [512, S], BF16)
    rg_d = scratch("rg", [512, S], BF16)
    rkt_d = scratch("rkt", [S, 512], BF16)
    rv_d = scratch("rv", [S, 512], BF16)
    oT_d = scratch("oT", [2048, S], BF16)
    mods_d = scratch("mods", [L, 128, 48], F32) if "mods" in dbg else None

    def fm(ap):
        return ap.rearrange("(k p) t -> p k t", p=128)

    C = {}

    def load_const(k):
        shp, dt = CONST_SPECS[k]
        b = P.sb(shp, dt, "c_" + k)
        LD(b, b[:], cin[k][:])
        return b

    for k in ("ident_f", "ident_b", "ones_f", "blkones_f"):
        C[k] = load_const(k)
    vec = P.sb([128, L, NV], F32, "vec")
    LD(vec, vec[:], vecs_in.rearrange("l p v -> p l v"))
    mod = P.sb([128, L, 48], F32, "mod")
    A1 = P.sb([128, L, 8], F32, "A1")
    A2 = P.sb([128, L, 8], F32, "A2")
    lbv = P.sb([128, L, 4], F32, "lbv")
    oml = P.sb([128, L, 4], F32, "oml")

    def V(l, name, j=0, n=1):
        o = VOFF[name] + j
        return vec[:, l, o:o + n]

    with P.scope():
        csb = P.sb([128, KD], F32, "csb")
        sc2 = P.sb([128, KD, 2], F32, "sc2")
        LD(csb, csb[:], c_in[:])
        ACT(sc2[:, :, 0], csb[:], AF.Silu, r=[csb], w=[sc2])
        ACT(sc2[:, :, 1], csb[:], AF.Silu, r=[csb], w=[sc2])
        aw = [P.sb([128, KD, 1536], F32, f"aw{i}") for i in range(2)]
        mps = P.ps([128, 512], F32, "mps")
        it = 0
        for l in range(L):
            for g in range(4):
                a = aw[it % 2]
                it += 1
                for k in range(KD):
                    LD(a, a[:, k, :], ada_w[l, k * 128:(k + 1) * 128, g * 1536:(g + 1) * 1536])
                for jj in range(12):
                    j = g * 12 + jj
                    for k in range(KD):
                        MM(mps[:, 2 * j:2 * j + 2], a[:, k, jj * 128:(jj + 1) * 128], sc2[:, k, :],
                           start=(k == 0), stop=(k == KD - 1), r=[a, sc2], w=[mps])
            TT("dve", mod[:, l, :], mps[:, 0:96].rearrange("p (j two) -> p j two", two=2)[:, :, 0],
               V(l, "adab", 0, 48), ALU.add, r=[mps, vec], w=[mod])
            STT(A1[:, l, :], mod[:, l, 8:16], 1.0, V(l, "n1g", 0, 8), ALU.add, ALU.mult, r=[mod, vec], w=[A1])
            STT(A2[:, l, :], mod[:, l, 32:40], 1.0, V(l, "n2g", 0, 8), ALU.add, ALU.mult, r=[mod, vec], w=[A2])
            if mods_d is not None:
                ST(mods_d[l], mod, mod[:, l, :])
        ex = P.sb([128, L, 4], F32, "ex")
        ssum = P.sb([128, 4], F32, "ssum")
        ACT(ex[:], vec[:, :, VOFF["hlb"]:VOFF["hlb"] + 4], AF.Exp, r=[vec], w=[ex])
        CP("dve", ssum[:], ex[:, 0, :], r=[ex], w=[ssum])
        for l in range(1, L):
            TT("dve", ssum[:], ssum[:], ex[:, l, :], ALU.add, r=[ssum, ex], w=[ssum])
        OP("dve", lambda en: en.reciprocal(ssum[:], ssum[:]), r=[ssum], w=[ssum])
        OP("dve", lambda en: en.memset(lbv[:, 0, :], 0.0), w=[lbv])
        for l in range(1, L):
            TT("dve", ex[:, l, :], ex[:, l, :], ssum[:], ALU.mult, r=[ex, ssum], w=[ex])
            TT("dve", lbv[:, l, :], lbv[:, l - 1, :], ex[:, l, :], ALU.add, r=[lbv, ex], w=[lbv])
        TS("dve", oml[:], lbv[:], -1.0, 1.0, ALU.mult, ALU.add, r=[lbv], w=[oml])

    def phase_A(l, x_src):
        with P.scope():
            w = P.sb([128, KD, IN_COLS], BF16, "w_in")
            for k in range(KD):
                LD(w, w[:, k, :], w_in[l, k * 128:(k + 1) * 128, :], e="pool")
            xs = [P.sb([128, KD, 512], F32, f"xs{i}") for i in range(2)]
            hT = [P.sb([128, KD, 512], BF16, f"hT{i}") for i in range(2)]
            sqs = [P.sb([128, 512], F32, f"sq{i}") for i in range(2)]
            rstd = P.sb([128, 512], F32, "rstd")
            tmp = [P.sb([128, 512], F32, f"tmpa{i}") for i in range(2)]
            stg = [P.sb([128, 4, 512], BF16, f"stg{i}") for i in range(3)]
            stg32 = [P.sb([128, 4, 512], F32, f"stgf{i}") for i in range(1)]
            stt = [P.sb([128, 4, 512], BF16, f"stt{i}") for i in range(2)]
            pss = [P.ps([128, 512], F32, f"psa{i}") for i in range(7)]
            pstat = P.ps([128, 512], F32, "pstat")
            cnt = {"ps": 0, "stg": 0, "stt": 0, "ev": 0, "tmp": 0, "s32": 0}

            def nxt(key, lst):
                b = lst[cnt[key] % len(lst)]
                cnt[key] += 1
                return b

            for tb in range(cfg.NTB):
                tsl = slice(tb * 512, (tb + 1) * 512)
                x = xs[tb % 2]
                h = hT[tb % 2]
                LD(x, x[:], fm(x_src)[:, :, tsl])
                for k in range(KD):
                    sq = sqs[k % 2]
                    ACT(sq[:], x[:, k, :], AF.Square, r=[x], w=[sq])
                    MM(pstat[:], C["ones_f"][:], sq[:], start=(k == 0), stop=(k == KD - 1), r=[C["ones_f"], sq], w=[pstat])
                ACT(rstd[:], pstat[:], AF.Ln, bias=EPS, scale=1.0 / D, r=[pstat], w=[rstd])
                ACT(rstd[:], rstd[:], AF.Exp, scale=-0.5, r=[rstd], w=[rstd])
                for k in range(KD):
                    t_ = nxt("tmp", tmp)
                    TT("dve", t_[:], x[:, k, :], rstd[:], ALU.mult, r=[x, rstd], w=[t_])
                    ACT(h[:, k, :], t_[:], AF.Identity, bias=mod[:, l, k:k + 1], scale=A1[:, l, k:k + 1], r=[t_, mod, A1], w=[h])
                ST(fm(hT_d)[:, :, tsl], h, h[:])

                def proj_fm(c0):
                    p_ = nxt("ps", pss)
                    for k in range(KD):
                        MM(p_[:], w[:, k, c0:c0 + 128], h[:, k, :], start=(k == 0), stop=(k == KD - 1), r=[w, h], w=[p_])
                    return p_

                def simple_group(c0, dst, func):
                    s_ = nxt("stg", stg)
                    for j in range(4):
                        p_ = proj_fm(c0 + j * 128)
                        if func is None:
                            e = "act" if cnt["ev"] % 2 == 0 else "dve"
                            cnt["ev"] += 1
                            CP(e, s_[:, j, :], p_[:], r=[p_], w=[s_])
                        else:
                            ACT(s_[:, j, :], p_[:], func, r=[p_], w=[s_])
                    ST(fm(dst)[:, :, tsl], s_, s_[:])

                simple_group(0, hq_d, AF.Silu)
                simple_group(1536, hg_d, AF.Silu)
                s32 = nxt("s32", stg32)
                sk_ = nxt("stg", stg)
                for j in range(4):
                    p_ = proj_fm(512 + j * 128)
                    t_ = nxt("tmp", tmp)
                    ACT(t_[:], p_[:], AF.Sigmoid, r=[p_], w=[t_])
                    TS("dve", t_[:], t_[:], oml[:, l, j:j + 1], lbv[:, l, j:j + 1], ALU.mult, ALU.add, r=[t_, oml, lbv], w=[t_])
                    ACT(s32[:, j, :], t_[:], AF.Ln, r=[t_], w=[s32])
                    TS("dve", sk_[:, j, :], t_[:], -1.0, 1.0, ALU.mult, ALU.add, r=[t_], w=[sk_])
                ST(fm(hlf_d)[:, :, tsl], s32, s32[:])
                ST(fm(hk_d)[:, :, tsl], sk_, sk_[:])
                s_ = nxt("stg", stg)
                for j in range(4):
                    pa = proj_fm(2048 + j * 128)
                    pg = proj_fm(2560 + j * 128)
                    t_ = nxt("tmp", tmp)
                    ACT(t_[:], pg[:], AF.Sigmoid, r=[pg], w=[t_])
                    TT("dve", s_[:, j, :], pa[:], t_[:], ALU.mult, r=[pa, t_], w=[s_])
                ST(fm(cz_d)[:, :, tsl], s_, s_[:])
                simple_group(3072, sq_d, None)
                simple_group(3584, sk_d, None)
                simple_group(4608, rq_d, None)
                simple_group(5120, rk_d, None)
                simple_group(6144, rg_d, AF.Silu)
                for c0, dst in ((1024, hv_d), (4096, sv_d), (5120, rkt_d), (5632, rv_d)):
                    s_ = nxt("stt", stt)
                    for st in range(4):
                        p_ = nxt("ps", pss)
                        for k in range(KD):
                            MM(p_[:], h[:, k, st * 128:(st + 1) * 128], w[:, k, c0:c0 + 512], start=(k == 0), stop=(k == KD - 1), r=[w, h], w=[p_])
                        e = "act" if cnt["ev"] % 2 == 0 else "dve"
                        cnt["ev"] += 1
                        CP(e, s_[:, st, :], p_[:], r=[p_], w=[s_])
                    ST(dst[tsl, :].rearrange("(st p) c -> p st c", p=128), s_, s_[:])


    def phase_ret(l):
        with P.scope():
            cdec = load_const("ret_dec")
            cgp = load_const("ret_gp")
            ckd = load_const("ret_kd")
            rq = [P.sb([128, 4, 512], BF16, f"rq{i}") for i in range(2)]
            rk = [P.sb([128, 4, 512], BF16, f"rk{i}") for i in range(2)]
            rg = [P.sb([128, 4, 512], BF16, f"rg{i}") for i in range(2)]
            rkt = [P.sb([128, 4, 512], BF16, f"rkt{i}") for i in range(2)]
            rv = [P.sb([128, 4, 512], BF16, f"rv{i}") for i in range(2)]
            qd = P.sb([128, 4, 512], BF16, "qd")
            kdc = P.sb([128, 4, 512], BF16, "kdc")
            pt = [P.sb([128, 512], BF16, f"pt{h}") for h in range(4)]
            st32 = [P.sb([128, 128], F32, f"st32_{h}") for h in range(4)]
            stb = [[P.sb([128, 128], BF16, f"stb{h}_{i}") for i in range(2)] for h in range(4)]
            sqt = P.sb([128, 512], F32, "sqt")
            rs = P.sb([128, 512], F32, "rs")
            o1 = P.sb([128, 512], F32, "o1")
            osb = [P.sb([128, 4, 512], BF16, f"osb{i}") for i in range(2)]
            ps_o = [P.ps([128, 512], F32, f"pso{h}") for h in range(4)]
            ps_s = P.ps([128, 512], F32, "pss")
            ps_kv = [P.ps([128, 512], F32, f"pskv{i}") for i in range(2)]
            ps_ss = P.ps([128, 512], F32, "psss")
            gam128 = [float(np.exp(128.0 * np.log1p(-(2.0 ** (-5.0 - h))))) for h in range(4)]
            for h in range(4):
                OP("dve", lambda en, h=h: en.memset(st32[h][:], 0.0), w=[st32[h]])
            nkv = 0
            for tb in range(cfg.NTB):
                tsl = slice(tb * 512, (tb + 1) * 512)
                i = tb % 2
                LD(rq[i], rq[i][:], fm(rq_d)[:, :, tsl])
                LD(rk[i], rk[i][:], fm(rk_d)[:, :, tsl])
                LD(rg[i], rg[i][:], fm(rg_d)[:, :, tsl])
                LD(rkt[i], rkt[i][:], rkt_d[tsl, :].rearrange("(st p) c -> p st c", p=128))
                LD(rv[i], rv[i][:], rv_d[tsl, :].rearrange("(st p) c -> p st c", p=128))
                TT("dve", qd[:], rq[i][:], cgp[:], ALU.mult, r=[rq[i], cgp], w=[qd])
                for c in range(4):
                    TT("pool", kdc[:, c, :], rkt[i][:, c, :], ckd[:], ALU.mult, r=[rkt[i], ckd], w=[kdc])
                for h in range(4):
                    for c in range(4):
                        cs = slice(c * 128, (c + 1) * 128)
                        MM(ps_s[:, cs], rk[i][:, h, cs], rq[i][:, h, cs], r=[rk[i], rq[i]], w=[ps_s])
                    TT("dve", pt[h][:], ps_s[:], cdec[:, h, :], ALU.mult, r=[ps_s, cdec], w=[pt[h]])
                for c in range(4):
                    cs = slice(c * 128, (c + 1) * 128)
                    first = (tb == 0 and c == 0)
                    for h in range(4):
                        hs = slice(h * 128, (h + 1) * 128)
                        sb_ = stb[h][(tb * 4 + c) % 2]
                        MM(ps_o[h][:, cs], rv[i][:, c, hs], pt[h][:, cs], start=True, stop=first, r=[rv[i], pt[h]], w=[ps_o[h]])
                        if not first:
                            MM(ps_o[h][:, cs], sb_[:], qd[:, h, cs], start=False, stop=True, r=[sb_, qd], w=[ps_o[h]])
                        pk = ps_kv[nkv % 2]
                        nkv += 1
                        MM(pk[:, 0:128], kdc[:, c, hs], rv[i][:, c, hs], r=[kdc, rv[i]], w=[pk])
                        nb_ = stb[h][(tb * 4 + c + 1) % 2]
                        STT(nb_[:], st32[h][:], gam128[h], pk[:, 0:128], ALU.mult, ALU.add, r=[st32[h], pk], w=[nb_])
                        STT(st32[h][:], st32[h][:], gam128[h], pk[:, 0:128], ALU.mult, ALU.add, r=[st32[h], pk], w=[st32[h]])
                ob = osb[tb % 2]
                for h in range(4):
                    ACT(sqt[:], ps_o[h][:], AF.Square, r=[ps_o[h]], w=[sqt])
                    MM(ps_ss[:], C["ones_f"][:], sqt[:], r=[C["ones_f"], sqt], w=[ps_ss])
                    ACT(rs[:], ps_ss[:], AF.Ln, bias=EPS, scale=1.0 / 128, r=[ps_ss], w=[rs])
                    ACT(rs[:], rs[:], AF.Exp, scale=-0.5, r=[rs], w=[rs])
                    TT("dve", o1[:], ps_o[h][:], rs[:], ALU.mult, r=[ps_o[h], rs], w=[o1])
                    STT(ob[:, h, :], o1[:], V(l, "rtg", h), rg[i][:, h, :], ALU.mult, ALU.mult, r=[o1, vec, rg[i]], w=[ob])
                ST(fm(oT_d)[:, 12:16, tsl], ob, ob[:])

    def phase_conv(l):
        with P.scope():
            dg = P.sb([128, 124, 128], BF16, "dg")
            for kk in range(124):
                TS("dve" if kk % 2 == 0 else "pool", dg[:, kk, :], C["ident_b"][:], V(l, "cvw", kk), None, ALU.mult, r=[C["ident_b"], vec], w=[dg])
            z = [P.sb([128, 4, 542], BF16, f"z{i}") for i in range(2)]
            ysb = P.sb([128, 4, 512], F32, "ysb")
            ysq = [P.sb([128, 512], F32, f"ysq{i}") for i in range(2)]
            mean = P.sb([128, 512], F32, "mean")
            msq = P.sb([128, 512], F32, "msq")
            rs = P.sb([128, 512], F32, "rs")
            t1 = [P.sb([128, 512], F32, f"t1{i}") for i in range(2)]
            osb = [P.sb([128, 4, 512], BF16, f"osb{i}") for i in range(2)]
            pc = [P.ps([128, 512], F32, f"pc{i}") for i in range(2)]
            p1 = P.ps([128, 512], F32, "p1")
            p2 = P.ps([128, 512], F32, "p2")
            for tb in range(cfg.NTB):
                tsl = slice(tb * 512, (tb + 1) * 512)
                zz = z[tb % 2]
                if tb == 0:
                    OP("dve", lambda en, zz=zz: en.memset(zz[:, :, 0:30], 0.0), w=[zz])
                    LD(zz, zz[:, :, 30:542], fm(cz_d)[:, :, tsl])
                else:
                    LD(zz, zz[:], fm(cz_d)[:, :, tb * 512 - 30:(tb + 1) * 512])
                for ch in range(4):
                    p_ = pc[ch % 2]
                    for k in range(31):
                        MM(p_[:], dg[:, k * 4 + ch, :], zz[:, ch, k:k + 512], start=(k == 0), stop=(k == 30), r=[dg, zz], w=[p_])
                    ACT(ysb[:, ch, :], p_[:], AF.Identity, bias=V(l, "cvb", ch), r=[p_, vec], w=[ysb])
                    q_ = ysq[ch % 2]
                    ACT(q_[:], p_[:], AF.Square, bias=V(l, "cvb", ch), r=[p_, vec], w=[q_])
                    MM(p1[:], C["ones_f"][:], ysb[:, ch, :], start=(ch == 0), stop=(ch == 3), r=[C["ones_f"], ysb], w=[p1])
                    MM(p2[:], C["ones_f"][:], q_[:], start=(ch == 0), stop=(ch == 3), r=[C["ones_f"], q_], w=[p2])
                ACT(mean[:], p1[:], AF.Identity, scale=1.0 / 512, r=[p1], w=[mean])
                TT("dve", msq[:], mean[:], mean[:], ALU.mult, r=[mean], w=[msq])
                STT(rs[:], p2[:], 1.0 / 512, msq[:], ALU.mult, ALU.subtract, r=[p2, msq], w=[rs])
                ACT(rs[:], rs[:], AF.Ln, bias=EPS, r=[rs], w=[rs])
                ACT(rs[:], rs[:], AF.Exp, scale=-0.5, r=[rs], w=[rs])
                ob = osb[tb % 2]
                for ch in range(4):
                    t_ = t1[ch % 2]
                    TT("dve", t_[:], ysb[:, ch, :], mean[:], ALU.subtract, r=[ysb, mean], w=[t_])
                    TT("dve", t_[:], t_[:], rs[:], ALU.mult, r=[t_, rs], w=[t_])
                    ACT(ob[:, ch, :], t_[:], AF.Silu, bias=V(l, "lnb", ch), scale=V(l, "lng", ch), r=[t_, vec], w=[ob])
                ST(fm(oT_d)[:, 4:8, tsl], ob, ob[:])

    def phase_hgrn(l):
        SBK = min(1024, S)
        NTT = SBK // 128
        CK = HG_CHK
        NCH = SBK // CK
        CPT = 128 // CK
        NHD = 4
        with P.scope():
            cmask = load_const("hg_mask")
            cscan = load_const("hg_scanmask")
            lf = P.sb([128, SBK], F32, "lf")
            qh = P.sb([128, SBK], BF16, "qh")
            kh = P.sb([128, SBK], BF16, "kh")
            b = P.sb([128, SBK], F32, "b")
            e1 = P.sb([128, SBK], F32, "e1")
            e2 = P.sb([128, SBK], F32, "e2")
            ks = P.sb([128, SBK], BF16, "ks")
            gh = [P.sb([128, SBK], BF16, f"gh{h}") for h in range(NHD)]
            vh = [P.sb([128, NTT, 128], BF16, f"vh{h}") for h in range(NHD)]
            vh32 = [P.sb([CK, NCH, 128], BF16, f"vh32{h}") for h in range(NHD)]
            qd = [P.sb([128, SBK], BF16, f"qd{h}") for h in range(NHD)]
            qd32 = [P.sb([128, SBK], F32, f"qd32{h}") for h in range(NHD)]
            kt = [P.sb([128, SBK], BF16, f"kt{h}") for h in range(NHD)]
            kst = [P.sb([CK, NCH, 128], BF16, f"kst{h}") for h in range(NHD)]
            dcc = P.sb([128, NCH, NHD], F32, "dcc")
            sdec = P.sb([128, NHD, 128], F32, "sdec")
            stb = [P.sb([128, NHD, 128], F32, f"stb{i}") for i in range(2)]
            ptb = [P.sb([128, NHD, 128], BF16, f"ptb{i}") for i in range(2)]
            sqt = P.sb([128, 512], F32, "sqt")
            rs = P.sb([128, 512], F32, "rs")
            o1 = P.sb([128, 512], F32, "o1")
            osb = [P.sb([128, 512], BF16, f"osb{i}") for i in range(2)]
            po = [P.ps([128, 512], F32, f"pso{h}") for h in range(NHD)]
            ps_t = P.ps([128, 512], BF16, "pst")
            ps_s = P.ps([128, 512], F32, "pss")
            ps_kv = P.ps([128, 512], F32, "pskv")
            ps_ss = P.ps([128, 512], F32, "psss")
            no = 0
            nst = 0
            OP("dve", lambda en: en.memset(stb[0][:], 0.0), w=[stb[0]])
            for sbk in range(S // SBK):
                csl = slice(sbk * SBK, (sbk + 1) * SBK)
                for h in range(NHD):
                    hs = slice(h * 128, (h + 1) * 128)
                    LD(lf, lf[:], hlf_d[hs, csl])
                    LD(qh, qh[:], hq_d[hs, csl])
                    LD(kh, kh[:], hk_d[hs, csl])
                    LD(gh[h], gh[h][:], hg_d[hs, csl])
                    LD(vh[h], vh[h][:], hv_d[csl, hs].rearrange("(tt p) c -> p tt c", p=128))
                    LD(vh32[h], vh32[h][:], hv_d[csl, hs].rearrange("(cc p) c -> p cc c", p=CK))
                    OP("dve", lambda en: en.tensor_tensor_scan(b[:], cscan[:, 0:SBK], lf[:], 0.0, ALU.mult, ALU.add), r=[cscan, lf], w=[b])
                    TS("dve", b[:], b[:], -75.0, None, ALU.max, r=[b], w=[b])
                    ACT(e1[:], b[:], AF.Exp, r=[b], w=[e1])
                    ACT(e2[:], b[:], AF.Exp, scale=-1.0, r=[b], w=[e2])
                    TT("dve", qd32[h][:], qh[:], e1[:], ALU.mult, r=[qh, e1], w=[qd32[h]])
                    CP("pool", qd[h][:], qd32[h][:], r=[qd32[h]], w=[qd[h]])
                    TT("pool", kt[h][:], kh[:], e2[:], ALU.mult, r=[kh, e2], w=[kt[h]])
                    b3 = b[:].rearrange("p (c j) -> p c j", j=CK)
                    ACT(dcc[:, :, h], b3[:, :, CK - 1], AF.Exp, r=[b], w=[dcc])
                    TT("dve", e1[:].rearrange("p (c j) -> p c j", j=CK), b3[:, :, CK - 1:CK].to_broadcast([128, NCH, CK]), b3,
                       ALU.subtract, r=[b, e1], w=[e1])
                    ACT(e1[:], e1[:], AF.Exp, r=[e1], w=[e1])
                    TT("pool", ks[:], kh[:], e1[:], ALU.mult, r=[kh, e1], w=[ks])
                    for c4 in range(NCH // 4):
                        for j in range(4):
                            ci = c4 * 4 + j
                            TR(ps_t[0:CK, j * 128:(j + 1) * 128], ks[:, ci * CK:(ci + 1) * CK], C["ident_b"][:], r=[ks, C["ident_b"]], w=[ps_t])
                        CP("act", kst[h][:, c4 * 4:(c4 + 1) * 4, :], ps_t[0:CK, :].rearrange("p (j c) -> p j c", c=128), r=[ps_t], w=[kst[h]])
                for t4 in range(NTT // 4):
                    for j in range(4):
                        tt = t4 * 4 + j
                        tl = slice(tt * 128, (tt + 1) * 128)
                        js = slice(j * 128, (j + 1) * 128)
                        pb = ptb[tt % 2]
                        for h in range(NHD):
                            MM(ps_s[:, h * 128:(h + 1) * 128], kt[h][:, tl], qd[h][:, tl], r=[kt[h], qd[h]], w=[ps_s])
                        TT("dve", pb[:].rearrange("p h t -> p (h t)"), ps_s[:], cmask[:], ALU.mult, r=[ps_s, cmask], w=[pb])
                        for h in range(NHD):
                            MM(po[h][:, js], vh[h][:, tt, :], pb[:, h, :], start=True, stop=False, r=[vh[h], pb], w=[po[h]])
                        for c in range(CPT):
                            ci = tt * CPT + c
                            cur = stb[nst % 2]
                            nx_ = stb[(nst + 1) % 2]
                            nst += 1
                            for h in range(NHD):
                                MM(po[h][:, j * 128 + c * CK:j * 128 + (c + 1) * CK], cur[:, h, :], qd32[h][:, tt * 128 + c * CK:tt * 128 + (c + 1) * CK],
                                   start=False, stop=(c == CPT - 1), r=[cur, qd32[h]], w=[po[h]])
                                MM(ps_kv[:, h * 128:(h + 1) * 128], kst[h][:, ci, :], vh32[h][:, ci, :], r=[kst[h], vh32[h]], w=[ps_kv])
                            TT("dve", sdec[:], cur[:], dcc[:, ci, :].unsqueeze(2).to_broadcast([128, NHD, 128]), ALU.mult, r=[cur, dcc], w=[sdec])
                            kv3 = ps_kv[:].rearrange("p (h v) -> p h v", v=128)
                            TT("dve", nx_[:], sdec[:], kv3, ALU.add, r=[sdec, ps_kv], w=[nx_])
                    c5 = slice(t4 * 512, (t4 + 1) * 512)
                    for h in range(NHD):
                        ACT(sqt[:], po[h][:], AF.Square, r=[po[h]], w=[sqt])
                        MM(ps_ss[:], C["ones_f"][:], sqt[:], r=[C["ones_f"], sqt], w=[ps_ss])
                        ACT(rs[:], ps_ss[:], AF.Ln, bias=EPS, scale=1.0 / 128, r=[ps_ss], w=[rs])
                        ACT(rs[:], rs[:], AF.Exp, scale=-0.5, r=[rs], w=[rs])
                        TT("dve", o1[:], po[h][:], rs[:], ALU.mult, r=[po[h], rs], w=[o1])
                        ob = osb[no % 2]
                        no += 1
                        STT(ob[:], o1[:], V(l, "hgng", h), gh[h][:, c5], ALU.mult, ALU.mult, r=[o1, vec, gh[h]], w=[ob])
                        ST(oT_d[h * 128:(h + 1) * 128, sbk * SBK + t4 * 512:sbk * SBK + (t4 + 1) * 512], ob, ob[:])

    def phase_sb(l):
        NT = cfg.NT
        QW = 1024
        NH = QW // 512
        NJ = QW // 128
        with P.scope():
            ctri = load_const("sb_trineg")
            cones = load_const("sb_onesneg")
            cdm = load_const("sb_dmask")
            g8 = P.sb([128, 2], F32, "g8")
            TS("dve", g8[:, 0:1], V(l, "qng"), 0.125, None, ALU.mult, r=[vec], w=[g8])
            CP("dve", g8[:, 1:2], V(l, "kng"), r=[vec], w=[g8])
            qraw = P.sb([128, S], BF16, "qraw")
            qn = P.sb([128, S], BF16, "qn")
            kn = P.sb([128, S], BF16, "kn")
            vv = P.sb([128, NT, 128], BF16, "vv")
            sqt = [P.sb([128, 512], F32, f"sqt{i}") for i in range(2)]
            rs = [P.sb([128, 512], F32, f"rs{i}") for i in range(2)]
            et = [P.sb([128, QW], F32, f"et{i}") for i in range(3)]
            exb = [P.sb([128, QW], F32, f"exb{i}") for i in range(2)]
            spb = [P.sb([128, QW], BF16, f"spb{i}") for i in range(3)]
            wb = [P.sb([128, QW], BF16, f"wb{i}") for i in range(3)]
            accb = [P.sb([128, QW], BF16, f"acc{i}") for i in range(3)]
            osb = [P.sb([64, QW], BF16, f"osb{i}") for i in range(2)]
            ps_z = [P.ps([128, QW], F32, f"psz{i}") for i in range(2)]
            ps_c = P.ps([128, QW], F32, "psc")
            ps_o = P.ps([128, QW], F32, "pso")
            n = {"pair": 0, "o": 0}
            for hp in range(4):
                rows = slice(hp * 128, (hp + 1) * 128)
                LD(vv, vv[:], sv_d[:, rows].rearrange("(tt p) c -> p tt c", p=128))
                for src, dst, gi in ((sq_d, qn, 0), (sk_d, kn, 1)):
                    LD(qraw, qraw[:], src[rows, :])
                    for pc_ in range(S // 512):
                        cs = slice(pc_ * 512, (pc_ + 1) * 512)
                        q_ = sqt[pc_ % 2]
                        r_ = rs[pc_ % 2]
                        ACT(q_[:], qraw[:, cs], AF.Square, r=[qraw], w=[q_])
                        MM(ps_z[0][:, 0:512], C["blkones_f"][:], q_[:], r=[C["blkones_f"], q_], w=[ps_z[0]])
                        ACT(r_[:], ps_z[0][:, 0:512], AF.Ln, bias=EPS, scale=1.0 / 64, r=[ps_z[0]], w=[r_])
                        ACT(r_[:], r_[:], AF.Exp, scale=-0.5, r=[r_], w=[r_])
                        STT(dst[:, cs], qraw[:, cs], g8[:, gi:gi + 1], r_[:], ALU.mult, ALU.mult, r=[qraw, g8, r_], w=[dst])
                pairs = []
                for a in range(2):
                    for qt in range(S // QW):
                        kbs = list(range(NJ * qt + NJ - 1, -1, -1))
                        for idx, kb in enumerate(kbs):
                            pairs.append(dict(a=a, qt=qt, kb=kb, idx=idx, last=(idx == len(kbs) - 1), j=kb - NJ * qt))
                NP = len(pairs)

                def S1(p):
                    d = pairs[p]
                    po_ = slice(d["a"] * 64, (d["a"] + 1) * 64)
                    ks_ = slice(d["kb"] * 128, (d["kb"] + 1) * 128)
                    pz = ps_z[p % 2]
                    e_ = et[p % 3]
                    sp = spb[p % 3]
                    for hh in range(NH):
                        hs_ = slice(hh * 512, (hh + 1) * 512)
                        qs = slice(d["qt"] * QW + hh * 512, d["qt"] * QW + (hh + 1) * 512)
                        MM(pz[:, hs_], kn[po_, ks_], qn[po_, qs], r=[kn, qn], w=[pz])
                    ACT(e_[:], pz[:], AF.Exp, r=[pz], w=[e_])
                    ACT(sp[:], e_[:], AF.Ln, bias=1.0, r=[e_], w=[sp])
                    if d["j"] >= 0:
                        TT("pool", sp[:], sp[:], cdm[:, d["j"], :], ALU.mult, r=[sp, cdm], w=[sp])
                    if not d["last"]:
                        nacc = accb[(p + 1) % 3]
                        if d["idx"] == 0:
                            CP("dve", nacc[:], sp[:], r=[sp], w=[nacc])
                        else:
                            TT("dve", nacc[:], accb[p % 3][:], sp[:], ALU.add, r=[accb[p % 3], sp], w=[nacc])

                def S2(p):
                    d = pairs[p]
                    po_ = slice(d["a"] * 64, (d["a"] + 1) * 64)
                    ks_ = slice(d["kb"] * 128, (d["kb"] + 1) * 128)
                    sp = spb[p % 3]
                    w_ = wb[p % 3]
                    acc = None if d["idx"] == 0 else accb[p % 3]
                    e_ = et[p % 3]
                    ex = exb[p % 2]
                    for hh in range(NH):
                        hs_ = slice(hh * 512, (hh + 1) * 512)
                        MM(ps_c[:, hs_], ctri[:], sp[:, hs_], start=True, stop=(acc is None), r=[ctri, sp], w=[ps_c])
                        if acc is not None:
                            MM(ps_c[:, hs_], cones[:], acc[:, hs_], start=False, stop=True, r=[cones, acc], w=[ps_c])
                    ACT(ex[:], ps_c[:], AF.Exp, r=[ps_c], w=[ex])
                    if d["j"] >= 0:
                        TT("dve", ex[:], ex[:], e_[:], ALU.mult, r=[ex, e_], w=[ex])
                        TT("pool", w_[:], ex[:], cdm[:, d["j"], :], ALU.mult, r=[ex, cdm], w=[w_])
                    else:
                        TT("dve", w_[:], ex[:], e_[:], ALU.mult, r=[ex, e_], w=[w_])

                def S3(p):
                    d = pairs[p]
                    po_ = slice(d["a"] * 64, (d["a"] + 1) * 64)
                    w_ = wb[p % 3]
                    for hh in range(NH):
                        hs_ = slice(hh * 512, (hh + 1) * 512)
                        MM(ps_o[0:64, hs_], vv[:, d["kb"], po_], w_[:, hs_], start=(d["idx"] == 0), stop=d["last"], r=[vv, w_], w=[ps_o])
                    if d["last"]:
                        ob = osb[n["o"] % 2]
                        n["o"] += 1
                        CP("dve", ob[:], ps_o[0:64, :], r=[ps_o], w=[ob])
                        r0 = 1024 + hp * 128 + d["a"] * 64
                        ST(oT_d[r0:r0 + 64, d["qt"] * QW:(d["qt"] + 1) * QW], ob, ob[:])

                for t in range(NP + 2):
                    if t < NP:
                        S1(t)
                    if 0 <= t - 1 < NP:
                        S2(t - 1)
                    if 0 <= t - 2 < NP:
                        S3(t - 2)

    def phase_C(l, x_src, x_dst):
        with P.scope():
            wg = P.sb([128, KD, 4096], BF16, "wg")
            wbr = P.sb([128, 16, 1024], BF16, "wbr")
            wo = P.sb([128, KD, 1024], BF16, "wo")
            for k in range(KD):
                LD(wg, wg[:, k, :], w_gate[l, k * 128:(k + 1) * 128, :], e="pool")
            for r_ in range(16):
                LD(wbr, wbr[:, r_, :], w_branch[l, r_ * 128:(r_ + 1) * 128, :], e="pool")
            for k in range(KD):
                LD(wo, wo[:, k, :], w_out[l, k * 128:(k + 1) * 128, :], e="pool")
            hT = [P.sb([128, KD, 512], BF16, f"hT{i}") for i in range(2)]
            oT = [P.sb([128, 16, 512], BF16, f"oT{i}") for i in range(2)]
            xs = P.sb([128, KD, 512], F32, "xs")
            gs = [P.sb([128, 512], F32, f"gs{i}") for i in range(2)]
            tmp = [P.sb([128, 512], F32, f"tmp{i}") for i in range(2)]
            m32 = P.sb([128, 512], F32, "m32")
            mg = P.sb([128, KD, 512], BF16, "mg")
            pg = [P.ps([128, 512], F32, f"pg{i}") for i in range(2)]
            py = [P.ps([128, 512], F32, f"py{i}") for i in range(2)]
            po = [P.ps([128, 512], F32, f"po{i}") for i in range(2)]

            def loads(tb):
                tsl = slice(tb * 512, (tb + 1) * 512)
                LD(hT[tb % 2], hT[tb % 2][:], fm(hT_d)[:, :, tsl])
                LD(oT[tb % 2], oT[tb % 2][:], fm(oT_d)[:, :, tsl])

            loads(0)
            it = 0
            for tb in range(cfg.NTB):
                tsl = slice(tb * 512, (tb + 1) * 512)
                if tb + 1 < cfg.NTB:
                    loads(tb + 1)
                LD(xs, xs[:], fm(x_src)[:, :, tsl])
                h = hT[tb % 2]
                o = oT[tb % 2]
                for dc in range(KD):
                    for n in range(4):
                        g_ = pg[it % 2]
                        y_ = py[it % 2]
                        s_ = gs[it % 2]
                        t_ = tmp[it % 2]
                        it += 1
                        c0 = n * 1024 + dc * 128
                        for k in range(KD):
                            MM(g_[:], wg[:, k, c0:c0 + 128], h[:, k, :], start=(k == 0), stop=(k == KD - 1), r=[wg, h], w=[g_])
                        for r_ in range(4):
                            MM(y_[:], wbr[:, n * 4 + r_, dc * 128:(dc + 1) * 128], o[:, n * 4 + r_, :], start=(r_ == 0), stop=(r_ == 3), r=[wbr, o], w=[y_])
                        ACT(s_[:], g_[:], AF.Sigmoid, bias=V(l, "bgate", n * 8 + dc), r=[g_, vec], w=[s_])
                        if n == 0:
                            TT("dve", m32[:], y_[:], s_[:], ALU.mult, r=[y_, s_], w=[m32])
                        else:
                            TT("dve", t_[:], y_[:], s_[:], ALU.mult, r=[y_, s_], w=[t_])
                            if n < 3:
                                TT("pool", m32[:], m32[:], t_[:], ALU.add, r=[m32, t_], w=[m32])
                            else:
                                TT("pool", mg[:, dc, :], m32[:], t_[:], ALU.add, r=[m32, t_], w=[mg])
                for dc in range(KD):
                    p_ = po[dc % 2]
                    t_ = tmp[dc % 2]
                    for k in range(KD):
                        MM(p_[:], wo[:, k, dc * 128:(dc + 1) * 128], mg[:, k, :], start=(k == 0), stop=(k == KD - 1), r=[wo, mg], w=[p_])
                    ACT(t_[:], p_[:], AF.Identity, scale=mod[:, l, 16 + dc:17 + dc], r=[p_, mod], w=[t_])
                    TT("dve", xs[:, dc, :], xs[:, dc, :], t_[:], ALU.add, r=[xs, t_], w=[xs])
                ST(fm(x_dst)[:, :, tsl], xs, xs[:])

    gdbg_d = scratch("gdbg", [S, 32], F32) if "gdbg" in dbg else None
    TBK = min(2048, S)
    NTI = TBK // 128
    NSB = TBK // 512

    def phase_D(l, x_src, x_dst):
        for blk in range(S // TBK):
            t0 = blk * TBK
            with P.scope():
                h2T = P.sb([128, KD, TBK], BF16, "h2T")
                G = P.sb([128, NTI, 32], F32, "G")
                acc = P.sb([128, NTI, 1024], F32, "acc")
                with P.scope():
                    rw = P.sb([128, KD, 36], F32, "rw")
                    rb = P.sb([128, 36], F32, "rb")
                    LD(rw, rw[:], rw_in[l].rearrange("(k p) c -> p k c", p=128))
                    LD(rb, rb[:], rb_in[l])
                    xs = [P.sb([128, KD, 512], F32, f"xs{i}") for i in range(2)]
                    sqs = [P.sb([128, 512], F32, f"sq{i}") for i in range(2)]
                    rstd = P.sb([128, 512], F32, "rstd")
                    tmp = [P.sb([128, 512], F32, f"tmp{i}") for i in range(2)]
                    h2f = P.sb([128, KD, 512], F32, "h2f")
                    lg = P.sb([128, 4, 36], F32, "lg")
                    sm = {n_: P.sb([128, 4], F32, "r_" + n_) for n_ in ("m4", "s4", "gp", "m1", "m2", "r", "w1", "w2")}
                    e4 = P.sb([128, 4, 4], F32, "e4")
                    oh4 = P.sb([128, 4, 4], F32, "oh4")
                    t48 = P.sb([128, 4, 4, 8], F32, "t48")
                    sel8 = P.sb([128, 4, 8], F32, "sel8")
                    sel8b = P.sb([128, 4, 8], F32, "sel8b")
                    oh1 = P.sb([128, 4, 8], F32, "oh1")
                    oh2 = P.sb([128, 4, 8], F32, "oh2")
                    g8 = P.sb([128, 4, 8], F32, "g8")
                    pstat = P.ps([128, 512], F32, "pstat")
                    ps_r = P.ps([128, 512], F32, "psr")

                    def bc(ap, shape):
                        return ap.to_broadcast(shape)

                    def RED(out, in_, op, r, w):
                        OP("dve", lambda en: en.tensor_reduce(out, in_, AX.X, op), r=r, w=w)

                    LD(xs[0], xs[0][:], fm(x_src)[:, :, t0:t0 + 512])
                    for sbi in range(NSB):
                        x = xs[sbi % 2]
                        if sbi + 1 < NSB:
                            LD(xs[(sbi + 1) % 2], xs[(sbi + 1) % 2][:], fm(x_src)[:, :, t0 + (sbi + 1) * 512:t0 + (sbi + 2) * 512])
                        for k in range(KD):
                            sq = sqs[k % 2]
                            ACT(sq[:], x[:, k, :], AF.Square, r=[x], w=[sq])
                            MM(pstat[:], C["ones_f"][:], sq[:], start=(k == 0), stop=(k == KD - 1), r=[C["ones_f"], sq], w=[pstat])
                        ACT(rstd[:], pstat[:], AF.Ln, bias=EPS, scale=1.0 / D, r=[pstat], w=[rstd])
                        ACT(rstd[:], rstd[:], AF.Exp, scale=-0.5, r=[rstd], w=[rstd])
                        for k in range(KD):
                            t_ = tmp[k % 2]
                            TT("dve", t_[:], x[:, k, :], rstd[:], ALU.mult, r=[x, rstd], w=[t_])
                            ACT(h2f[:, k, :], t_[:], AF.Identity, bias=mod[:, l, 24 + k:25 + k], scale=A2[:, l, k:k + 1], r=[t_, mod, A2], w=[h2f])
                            CP("pool", h2T[:, k, sbi * 512:(sbi + 1) * 512], h2f[:, k, :], r=[h2f], w=[h2T])
                        for i in range(4):
                            for k in range(KD):
                                MM(ps_r[:, i * 36:(i + 1) * 36], h2f[:, k, i * 128:(i + 1) * 128], rw[:, k, :], start=(k == 0), stop=(k == KD - 1), r=[h2f, rw], w=[ps_r])
                        TT("dve", lg[:], ps_r[:, 0:144].rearrange("p (i c) -> p i c", c=36), rb[:].unsqueeze(1).to_broadcast([128, 4, 36]), ALU.add, r=[ps_r, rb], w=[lg])
                        lgg = lg[:, :, 0:4]
                        el = lg[:, :, 4:36].rearrange("p i (g e) -> p i g e", e=8)
                        RED(sm["m4"][:], lgg, ALU.max, [lg], [sm["m4"]])
                        TT("dve", oh4[:], lgg, bc(sm["m4"][:].unsqueeze(2), [128, 4, 4]), ALU.is_equal, r=[lg, sm["m4"]], w=[oh4])
                        TT("dve", e4[:], lgg, bc(sm["m4"][:].unsqueeze(2), [128, 4, 4]), ALU.subtract, r=[lg, sm["m4"]], w=[e4])
                        ACT(e4[:], e4[:], AF.Exp, r=[e4], w=[e4])
                        RED(sm["s4"][:], e4[:], ALU.add, [e4], [sm["s4"]])
                        OP("dve", lambda en: en.reciprocal(sm["gp"][:], sm["s4"][:]), r=[sm["s4"]], w=[sm["gp"]])
                        TT("dve", t48[:], el, bc(oh4[:].unsqueeze(3), [128, 4, 4, 8]), ALU.mult, r=[lg, oh4], w=[t48])
                        RED(sel8[:], t48[:].rearrange("p i g e -> p i e g"), ALU.add, [t48], [sel8])
                        RED(sm["m1"][:], sel8[:], ALU.max, [sel8], [sm["m1"]])
                        TT("dve", oh1[:], sel8[:], bc(sm["m1"][:].unsqueeze(2), [128, 4, 8]), ALU.is_equal, r=[sel8, sm["m1"]], w=[oh1])
                        STT(sel8b[:], oh1[:], -1e30, sel8[:], ALU.mult, ALU.add, r=[oh1, sel8], w=[sel8b])
                        RED(sm["m2"][:], sel8b[:], ALU.max, [sel8b], [sm["m2"]])
                        TT("dve", oh2[:], sel8b[:], bc(sm["m2"][:].unsqueeze(2), [128, 4, 8]), ALU.is_equal, r=[sel8b, sm["m2"]], w=[oh2])
                        TT("dve", sm["r"][:], sm["m2"][:], sm["m1"][:], ALU.subtract, r=[sm["m2"], sm["m1"]], w=[sm["r"]])
                        ACT(sm["r"][:], sm["r"][:], AF.Exp, r=[sm["r"]], w=[sm["r"]])
                        TS("dve", sm["w1"][:], sm["r"][:], 1.0, None, ALU.add, r=[sm["r"]], w=[sm["w1"]])
                        OP("dve", lambda en: en.reciprocal(sm["w1"][:], sm["w1"][:]), r=[sm["w1"]], w=[sm["w1"]])
                        TT("dve", sm["w2"][:], sm["w1"][:], sm["r"][:], ALU.mult, r=[sm["w1"], sm["r"]], w=[sm["w2"]])
                        TT("dve", sm["w1"][:], sm["w1"][:], sm["gp"][:], ALU.mult, r=[sm["w1"], sm["gp"]], w=[sm["w1"]])
                        TT("dve", sm["w2"][:], sm["w2"][:], sm["gp"][:], ALU.mult, r=[sm["w2"], sm["gp"]], w=[sm["w2"]])
                        TT("dve", oh1[:], oh1[:], bc(sm["w1"][:].unsqueeze(2), [128, 4, 8]), ALU.mult, r=[oh1, sm["w1"]], w=[oh1])
                        TT("dve", oh2[:], oh2[:], bc(sm["w2"][:].unsqueeze(2), [128, 4, 8]), ALU.mult, r=[oh2, sm["w2"]], w=[oh2])
                        TT("dve", g8[:], oh1[:], oh2[:], ALU.add, r=[oh1, oh2], w=[g8])
                        TT("dve", G[:, sbi * 4:(sbi + 1) * 4, :].rearrange("p i (g e) -> p i g e", e=8), bc(oh4[:].unsqueeze(3), [128, 4, 4, 8]),
                           bc(g8[:].unsqueeze(2), [128, 4, 4, 8]), ALU.mult, r=[oh4, g8], w=[G])
                    if gdbg_d is not None:
                        ST(gdbg_d[t0:t0 + TBK, :].rearrange("(i p) c -> p i c", p=128), G, G[:])
                if getattr(cfg, "d_stage", 9) < 2:
                    continue
                with P.scope():
                    w1 = [P.sb([128, KD, 512], BF16, f"w1_{i}") for i in range(2)]
                    w3 = [P.sb([128, KD, 512], BF16, f"w3_{i}") for i in range(2)]
                    w2 = [P.sb([128, 4, 1024], BF16, f"w2_{i}") for i in range(2)]
                    aT = P.sb([128, 4, TBK], BF16, "aT")
                    sl = [P.sb([128, 512], F32, f"sl{i}") for i in range(2)]
                    yt = [P.sb([128, 1024], F32, f"yt{i}") for i in range(2)]
                    ph1 = [P.ps([128, 512], F32, f"ph1{i}") for i in range(2)]
                    ph3 = [P.ps([128, 512], F32, f"ph3{i}") for i in range(2)]
                    py = [P.ps([128, 1024], F32, f"py{i}") for i in range(2)]

                    def wloads(e):
                        i = e % 2
                        LD(w1[i], w1[i][:], ew1[l, e].rearrange("(k p) f -> p k f", p=128), e="pool")
                        LD(w3[i], w3[i][:], ew3[l, e].rearrange("(k p) f -> p k f", p=128), e="pool")
                        LD(w2[i], w2[i][:], ew2[l, e].rearrange("(k p) d -> p k d", p=128), e="pool")

                    wloads(0)
                    it = 0
                    iy = 0
                    for e in range(N_EXP):
                        i = e % 2
                        if e + 1 < N_EXP:
                            wloads(e + 1)
                        for sbi in range(NSB):
                            cs = slice(sbi * 512, (sbi + 1) * 512)
                            for fc in range(4):
                                a_ = ph1[it % 2]
                                b_ = ph3[it % 2]
                                s_ = sl[it % 2]
                                it += 1
                                for k in range(KD):
                                    MM(a_[:], w1[i][:, k, fc * 128:(fc + 1) * 128], h2T[:, k, cs], start=(k == 0), stop=(k == KD - 1), r=[w1[i], h2T], w=[a_])
                                for k in range(KD):
                                    MM(b_[:], w3[i][:, k, fc * 128:(fc + 1) * 128], h2T[:, k, cs], start=(k == 0), stop=(k == KD - 1), r=[w3[i], h2T], w=[b_])
                                ACT(s_[:], a_[:], AF.Silu, r=[a_], w=[s_])
                                TT("dve", aT[:, fc, cs], b_[:], s_[:], ALU.mult, r=[b_, s_], w=[aT])
                        for tt in range(NTI):
                            p_ = py[iy % 2]
                            y_ = yt[iy % 2]
                            iy += 1
                            for dh in range(2):
                                for fc in range(4):
                                    MM(p_[:, dh * 512:(dh + 1) * 512], aT[:, fc, tt * 128:(tt + 1) * 128], w2[i][:, fc, dh * 512:(dh + 1) * 512],
                                       start=(fc == 0), stop=(fc == 3), r=[aT, w2[i]], w=[p_])
                            if e == 0:
                                ACT(acc[:, tt, :], p_[:], AF.Identity, scale=G[:, tt, e:e + 1], r=[p_, G], w=[acc])
                            else:
                                ACT(y_[:], p_[:], AF.Identity, scale=G[:, tt, e:e + 1], r=[p_, G], w=[y_])
                                TT("dve" if iy % 2 else "pool", acc[:, tt, :], acc[:, tt, :], y_[:], ALU.add, r=[acc, y_], w=[acc])
                if getattr(cfg, "d_stage", 9) < 3:
                    continue
                with P.scope():
                    xs = [P.sb([128, KD, 512], F32, f"xo{i}") for i in range(2)]
                    tmp = [P.sb([128, 512], F32, f"tmpo{i}") for i in range(2)]
                    pt = [P.ps([128, 512], F32, f"pto{i}") for i in range(2)]
                    n_ = 0
                    for sbi in range(NSB):
                        x = xs[sbi % 2]
                        LD(x, x[:], fm(x_src)[:, :, t0 + sbi * 512:t0 + (sbi + 1) * 512])
                        for k in range(KD):
                            p_ = pt[n_ % 2]
                            t_ = tmp[n_ % 2]
                            n_ += 1
                            for j in range(4):
                                MM(p_[:, j * 128:(j + 1) * 128], acc[:, sbi * 4 + j, k * 128:(k + 1) * 128], C["ident_f"][:], r=[acc, C["ident_f"]], w=[p_])
                            ACT(t_[:], p_[:], AF.Identity, scale=mod[:, l, 40 + k:41 + k], r=[p_, mod], w=[t_])
                            TT("dve", x[:, k, :], x[:, k, :], t_[:], ALU.add, r=[x, t_], w=[x])
                        ST(fm(x_dst)[:, :, t0 + sbi * 512:t0 + (sbi + 1) * 512], x, x[:])

    return nc, P, dict(phase_A=phase_A, phase_ret=phase_ret, phase_conv=phase_conv, phase_hgrn=phase_hgrn, phase_sb=phase_sb,
                       phase_C=phase_C, phase_D=phase_D,
                       xT_in=xT_in, xA=xA, xB=xB, out_T=out_T)


def _chunkcols(v):
    return np.ascontiguousarray(np.asarray(v, np.float32).reshape(-1, 128).T)


def make_in_maps(inp, cfg):
    L = cfg.L
    B = inp["x"].shape[0]
    vecs = np.zeros((L, 128, NV), np.float32)
    for l in range(L):
        def put(name, arr):
            a = _chunkcols(arr)
            vecs[l][:, VOFF[name]:VOFF[name] + a.shape[1]] = a
        put("n1g", inp["norm1_g"][l]); put("n2g", inp["norm2_g"][l]); put("adab", inp["ada_b"][l])
        put("hgng", inp["hgrn_norm_g"][l]); put("cvb", inp["conv_b"][l]); put("lng", inp["conv_ln_g"][l])
        put("lnb", inp["conv_ln_b"][l]); put("rtg", inp["ret_norm_g"][l]); put("bgate", inp["b_gate"][l])
        put("hlb", inp["hgrn_lb"][l])
        vecs[l][:, VOFF["qng"]] = np.tile(np.asarray(inp["sb_qnorm_g"][l], np.float32), 2)
        vecs[l][:, VOFF["kng"]] = np.tile(np.asarray(inp["sb_knorm_g"][l], np.float32), 2)
        cw = np.asarray(inp["conv_w"][l], np.float32)
        for k in range(31):
            vecs[l][:, VOFF["cvw"] + 4 * k:VOFF["cvw"] + 4 * k + 4] = _chunkcols(cw[k])
    rw = np.ascontiguousarray(np.concatenate([np.asarray(inp["router_group_w"], np.float32)[:L],
                                              np.asarray(inp["router_expert_w"], np.float32)[:L]], axis=-1))
    rb1 = np.concatenate([np.asarray(inp["router_group_b"], np.float32)[:L], np.asarray(inp["router_expert_b"], np.float32)[:L]], axis=-1)
    rb = np.ascontiguousarray(np.broadcast_to(rb1[:, None, :], (L, 128, 36)))
    consts = host_consts()
    shared = {
        "vecs": vecs, "rw": rw, "rb": rb,
        "ada_w": np.ascontiguousarray(np.asarray(inp["ada_w"], np.float32)[:L]),
        "w_in": np.ascontiguousarray(np.asarray(inp["w_in"], np.float32)[:L]),
        "w_branch": np.ascontiguousarray(np.asarray(inp["w_branch"], np.float32)[:L]),
        "w_gate": np.ascontiguousarray(np.asarray(inp["w_gate"], np.float32)[:L]),
        "w_out": np.ascontiguousarray(np.asarray(inp["w_out"], np.float32)[:L]),
        "ew1": np.ascontiguousarray(np.asarray(inp["expert_w1"], np.float32)[:L]),
        "ew3": np.ascontiguousarray(np.asarray(inp["expert_w3"], np.float32)[:L]),
        "ew2": np.ascontiguousarray(np.asarray(inp["expert_w2"], np.float32)[:L]),
    }
    for k, v in consts.items():
        shared["k_" + k] = v
    maps = []
    for b in range(B):
        m = dict(shared)
        m["xT"] = np.ascontiguousarray(np.asarray(inp["x"][b], np.float32).T)
        m["c"] = _chunkcols(inp["c"][b])
        maps.append(m)
    return maps


def build_full(cfg):
    nc, P, ph = build_program(cfg)
    L = cfg.L
    src = ph["xT_in"]
    for l in range(L):
        ph["phase_A"](l, src)
        ph["phase_conv"](l)
        ph["phase_ret"](l)
        ph["phase_hgrn"](l)
        ph["phase_sb"](l)
        ph["phase_C"](l, src, ph["xA"])
        dst = ph["out_T"] if l == L - 1 else ph["xB"]
        ph["phase_D"](l, ph["xA"], dst)
        src = dst
    P.barrier()
    P.emit()
    return nc, P


def kernel(**inputs):
    x = np.asarray(inputs["x"])
    B, S, _ = x.shape
    L = np.asarray(inputs["w_in"]).shape[0]
    cfg = Cfg(S=S, L=L)
    nc, P = build_full(cfg)
    maps = make_in_maps(inputs, cfg)
    res = run_bass_kernel_spmd(nc, maps, core_ids=list(range(len(maps))))
    out = np.stack([np.ascontiguousarray(np.asarray(r["outT"]).T) for r in res.results], axis=0)
    return out.astype(np.float32)
```

```python
import numpy as np
from contextlib import ExitStack, contextmanager
import concourse.bass as bass
import concourse.mybir as mybir
from concourse.bass_utils import run_bass_kernel_spmd

F32 = mybir.dt.float32
BF16 = mybir.dt.bfloat16
AF = mybir.ActivationFunctionType
ALU = mybir.AluOpType
AX = mybir.AxisListType

D = 1024
KD = 8
IN_COLS = 6656
EPS = 1e-6
N_EXP = 32
D_EXP = 512
SAME_ENG_SYNC = True
SAME_ENG_MIN_FREE = 1 << 30
FRESH_SWDGE_SEM = False

VOFF = {}
_o = 0
for _n, _w in (("n1g", 8), ("n2g", 8), ("adab", 48), ("hgng", 4), ("cvb", 4), ("lng", 4), ("lnb", 4), ("qng", 1),
               ("kng", 1), ("rtg", 4), ("bgate", 32), ("cvw", 124), ("hlb", 4)):
    VOFF[_n] = _o
    _o += _w
NV = _o


class DSem:
    def __init__(self, sem, cnt=0):
        self.sem = sem
        self.cnt = cnt


class Buf:
    def __init__(self, t, name):
        self.t = t
        self.name = name
        n = 1
        for d_ in list(t.shape)[1:]:
            n *= int(d_)
        self.small = n < SAME_ENG_MIN_FREE
        self.last_w = None
        self.reads = {}
        self.dsem = None

    def __getitem__(self, idx):
        return self.t[idx]


class Prog:
    ENGS = ("pe", "act", "dve", "pool", "sp")

    def __init__(self, nc):
        self.nc = nc
        self.root = ExitStack()
        self.stacks = [self.root]
        self.eng = {"pe": nc.tensor, "act": nc.scalar, "dve": nc.vector, "pool": nc.gpsimd, "sp": nc.sync}
        self.q = {e: [] for e in self.ENGS}
        self.cnt = {e: 0 for e in self.ENGS}
        self.sem = {e: self.root.enter_context(nc.semaphore("s_" + e)) for e in self.ENGS}
        self.known = {e: {} for e in self.ENGS}
        self.nbuf = 0
        self.sempool = []
        self.scope_bufs = [[]]
        self.live_dsems = []
        self.ninstr = 0

    @contextmanager
    def scope(self):
        st = ExitStack()
        self.stacks.append(st)
        self.scope_bufs.append([])
        try:
            yield
        finally:
            self.barrier()
            for b in self.scope_bufs.pop():
                if b.dsem is not None:
                    self.sempool.append(b.dsem)
                    b.dsem = None
            self.stacks.pop()
            st.close()

    def _reg(self, t, name):
        b = Buf(t, name)
        self.scope_bufs[-1].append(b)
        return b

    def sb(self, shape, dt, name=None):
        self.nbuf += 1
        name = f"{name or 'sb'}_{self.nbuf}"
        return self._reg(self.stacks[-1].enter_context(self.nc.sbuf_tensor(name, list(shape), dt)), name)

    def ps(self, shape, dt, name=None):
        self.nbuf += 1
        name = f"{name or 'ps'}_{self.nbuf}"
        return self._reg(self.stacks[-1].enter_context(self.nc.psum_tensor(name, list(shape), dt)), name)

    def dram(self, name, shape, dt, kind="Internal"):
        return self.nc.dram_tensor(name, list(shape), dt, kind=kind).ap()

    def _wait(self, e, key, val, small=True):
        if key == e and (e == "pe" or not SAME_ENG_SYNC or not small):
            return
        if self.known[e].get(key, 0) >= val:
            return
        self.known[e][key] = val
        sem = self.sem[key] if isinstance(key, str) else key.sem
        self.q[e].append(lambda en, sem=sem, val=val: en.wait_ge(sem, val))
        self.ninstr += 1

    def _deps(self, e, reads, writes):
        for b in reads:
            if b.last_w is not None:
                self._wait(e, *b.last_w, small=b.small)
        for b in writes:
            if b.last_w is not None:
                self._wait(e, *b.last_w, small=b.small)
            for k, v in b.reads.items():
                self._wait(e, k, v, small=b.small)

    def _commit(self, ev, reads, writes):
        for b in reads:
            if b.reads.get(ev[0], 0) < ev[1]:
                b.reads[ev[0]] = ev[1]
        for b in writes:
            b.last_w = ev
            b.reads = {}

    def op(self, e, fn, reads=(), writes=()):
        self._deps(e, reads, writes)
        self.cnt[e] += 1
        ev = (e, self.cnt[e])
        sem = self.sem[e]
        self.q[e].append(lambda en, fn=fn, sem=sem: fn(en).then_inc(sem, 1))
        self.ninstr += 1
        self._commit(ev, reads, writes)

    def dma(self, e, out_ap, in_ap, reads=(), writes=(), owner=None, **kw):
        self._deps(e, reads, writes)
        if FRESH_SWDGE_SEM and e == "pool":
            owner.dsem = DSem(self.root.enter_context(self.nc.semaphore(f"f{len(self.live_dsems)}")))
            self.live_dsems.append(owner.dsem)
        if owner.dsem is None:
            if self.sempool:
                owner.dsem = self.sempool.pop()
            else:
                owner.dsem = DSem(self.root.enter_context(self.nc.semaphore(f"d{len(self.live_dsems)}")))
                self.live_dsems.append(owner.dsem)
        ds = owner.dsem
        ds.cnt += 16
        ev = (ds, ds.cnt)
        self.q[e].append(lambda en, o=out_ap, i=in_ap, sem=ds.sem, kw=kw: en.dma_start(out=o, in_=i, **kw).then_inc(sem, 16))
        self.ninstr += 1
        self._commit(ev, reads, writes)

    def barrier(self):
        evs = [(k, self.cnt[k]) for k in self.ENGS if self.cnt[k] > 0]
        evs += [(ds, ds.cnt) for ds in self.live_dsems if ds.cnt > 0]
        for e in self.ENGS:
            for k, v in evs:
                if k == e:
                    if e != "pe" and self.known[e].get(e, 0) < v:
                        self.known[e][e] = v
                        self.q[e].append(lambda en, sem=self.sem[e], val=v: en.wait_ge(sem, val))
                    continue
                self._wait(e, k, v)

    def emit(self):
        nc = self.nc
        with nc.Block() as block:
            @block.tensor
            def _(en):
                for f in self.q["pe"]:
                    f(en)

            @block.scalar
            def _(en):
                for f in self.q["act"]:
                    f(en)

            @block.vector
            def _(en):
                for f in self.q["dve"]:
                    f(en)

            @block.gpsimd
            def _(en):
                for f in self.q["pool"]:
                    f(en)

            @block.sync
            def _(en):
                for f in self.q["sp"]:
                    f(en)
        self.root.close()


def host_consts():
    import ml_dtypes
    bf = ml_dtypes.bfloat16
    c = {}
    c["ident_f"] = np.eye(128, dtype=np.float32)
    c["ident_b"] = np.eye(128).astype(bf)
    c["ones_f"] = np.ones((128, 128), np.float32)
    blk = np.zeros((128, 128), np.float32)
    blk[:64, :64] = 1
    blk[64:, 64:] = 1
    c["blkones_f"] = blk
    s = np.arange(128)[:, None].astype(np.float64)
    t = np.arange(128)[None, :].astype(np.float64)
    dec = np.zeros((128, 4, 512), np.float64)
    gp = np.zeros((128, 4, 512), np.float64)
    kd = np.zeros((128, 512), np.float64)
    for h in range(4):
        lg = np.log1p(-(2.0 ** (-5.0 - h)))
        m = np.where(t >= s, np.exp(lg * np.maximum(t - s, 0)), 0.0) * (128 ** -0.5)
        dec[:, h, :] = np.tile(m, (1, 4))
        gp[:, h, :] = np.tile(np.exp(lg * (np.arange(128) + 1.0))[None, :], (128, 4))
        kd[:, h * 128:(h + 1) * 128] = (np.exp(lg * (127.0 - np.arange(128))) * (128 ** -0.5))[:, None]
    c["ret_dec"] = dec.astype(np.float32)
    c["ret_gp"] = gp.astype(np.float32)
    c["ret_kd"] = kd.astype(np.float32)
    same = (np.arange(128)[:, None] // 32) == (np.arange(128)[None, :] // 32)
    m = (same & (np.arange(128)[:, None] <= np.arange(128)[None, :])).astype(np.float32)
    c["hg_mask"] = np.tile(m, (1, 4)).astype(np.float32)
    sm = np.ones((128, 2048), np.float32)
    sm[:, ::32] = 0.0
    c["hg_scanmask"] = sm
    c["sb_trineg"] = (-(np.arange(128)[:, None] >= np.arange(128)[None, :]).astype(np.float32)).astype(bf)
    c["sb_onesneg"] = (-np.ones((128, 128), np.float32)).astype(bf)
    dm = np.zeros((128, 8, 1024), np.float32)
    for j in range(8):
        dm[:, j, :] = ((128 * j + np.arange(128))[:, None] < np.arange(1024)[None, :]).astype(np.float32)
    c["sb_dmask"] = dm.astype(bf)
    c["moe_slt"] = (np.arange(128)[:, None] < np.arange(128)[None, :]).astype(np.float32).astype(bf)
    c["ones_b"] = np.ones((128, 128), np.float32).astype(bf)
    c["moe_iota"] = np.ascontiguousarray(np.broadcast_to((np.arange(64) - 1000.0).astype(np.float32)[None, :], (128, 64)))
    return c


CONST_SPECS = {
    "ident_f": ([128, 128], F32), "ident_b": ([128, 128], BF16), "ones_f": ([128, 128], F32),
    "blkones_f": ([128, 128], F32), "ret_dec": ([128, 4, 512], F32), "ret_gp": ([128, 4, 512], F32),
    "ret_kd": ([128, 512], F32), "hg_mask": ([128, 512], F32), "hg_scanmask": ([128, 2048], F32),
    "moe_slt": ([128, 128], BF16), "ones_b": ([128, 128], BF16), "moe_iota": ([128, 64], F32),
    "sb_trineg": ([128, 128], BF16), "sb_onesneg": ([128, 128], BF16), "sb_dmask": ([128, 8, 1024], BF16),
}


class Cfg:
    def __init__(self, S=8192, L=4, debug=()):
        self.S = S
        self.L = L
        self.NTB = S // 512
        self.NT = S // 128
        self.debug = set(debug)


def build_program(cfg):
    nc = bass.Bass("TRN2", target_bir_lowering=False)
    P = Prog(nc)
    S, L = cfg.S, cfg.L
    dbg = cfg.debug

    def OP(e, fn, r=(), w=()):
        P.op(e, fn, reads=r, writes=w)

    def MM(out, lhsT, rhs, start=True, stop=True, r=(), w=()):
        P.op("pe", lambda en: en.matmul(out, lhsT, rhs, start=start, stop=stop), reads=r, writes=w)

    def TR(out, in_, ident, r=(), w=()):
        P.op("pe", lambda en: en.transpose(out, in_, ident), reads=r, writes=w)

    def ACT(out, in_, func, bias=None, scale=None, r=(), w=(), accum=None):
        kw = {}
        if bias is not None:
            kw["bias"] = bias
        if scale is not None:
            kw["scale"] = scale
        if accum is not None:
            kw["accum_out"] = accum
        P.op("act", lambda en: en.activation(out, in_, func, **kw), reads=r, writes=w)

    def TT(e, out, in0, in1, op, r=(), w=()):
        P.op(e, lambda en: en.tensor_tensor(out, in0, in1, op), reads=r, writes=w)

    def TS(e, out, in0, s1, s2, op0, op1=None, r=(), w=()):
        if op1 is None:
            P.op(e, lambda en: en.tensor_scalar(out, in0, s1, None, op0), reads=r, writes=w)
        else:
            P.op(e, lambda en: en.tensor_scalar(out, in0, s1, s2, op0, op1), reads=r, writes=w)

    def STT(out, in0, scalar, in1, op0, op1, r=(), w=()):
        P.op("dve", lambda en: en.scalar_tensor_tensor(out, in0, scalar, in1, op0, op1), reads=r, writes=w)

    def CP(e, out, in_, r=(), w=()):
        if e == "act":
            P.op("act", lambda en: en.activation(out, in_, AF.Identity), reads=r, writes=w)
        else:
            P.op(e, lambda en: en.tensor_copy(out, in_), reads=r, writes=w)

    def LD(out_buf, out_ap, in_ap, e="sp"):
        P.dma(e, out_ap, in_ap, writes=[out_buf], owner=out_buf)

    def ST(out_ap, in_buf, in_ap, e="pool"):
        P.dma(e, out_ap, in_ap, reads=[in_buf], owner=in_buf)

    xT_in = P.dram("xT", [D, S], F32, kind="ExternalInput")
    c_in = P.dram("c", [128, KD], F32, kind="ExternalInput")
    vecs_in = P.dram("vecs", [L, 128, NV], F32, kind="ExternalInput")
    ada_w = P.dram("ada_w", [L, D, 6 * D], F32, kind="ExternalInput")
    w_in = P.dram("w_in", [L, D, IN_COLS], F32, kind="ExternalInput")
    w_branch = P.dram("w_branch", [L, 2048, D], F32, kind="ExternalInput")
    w_gate = P.dram("w_gate", [L, D, 4 * D], F32, kind="ExternalInput")
    w_out = P.dram("w_out", [L, D, D], F32, kind="ExternalInput")
    rw_in = P.dram("rw", [L, D, 36], F32, kind="ExternalInput")
    rb_in = P.dram("rb", [L, 128, 36], F32, kind="ExternalInput")
    ew1 = P.dram("ew1", [L, N_EXP, D, D_EXP], F32, kind="ExternalInput")
    ew3 = P.dram("ew3", [L, N_EXP, D, D_EXP], F32, kind="ExternalInput")
    ew2 = P.dram("ew2", [L, N_EXP, D_EXP, D], F32, kind="ExternalInput")
    cin = {k: P.dram("k_" + k, shp, dt, kind="ExternalInput") for k, (shp, dt) in CONST_SPECS.items()}
    out_T = P.dram("outT", [D, S], F32, kind="ExternalOutput")

    def scratch(name, shape, dt):
        return P.dram(name, shape, dt, kind=("ExternalOutput" if name in dbg else "Internal"))

    xA = scratch("xA", [D, S], F32)
    xB = scratch("xB", [D, S], F32)
    hT_d = scratch("hT", [D, S], BF16)
    hq_d = scratch("hq", [512, S], BF16)
    hlf_d = scratch("hlf", [512, S], F32)
    hk_d = scratch("hk", [512, S], BF16)
    hg_d = scratch("hg", [512, S], BF16)
    hv_d = scratch("hv", [S, 512], BF16)
    cz_d = scratch("cz", [512, S], BF16)
    sq_d = scratch("sq", [512, S], BF16)
    sk_d = scratch("sk", [512, S], BF16)
    sv_d = scratch("sv", [S, 512], BF16)
    rq_d = scratch("rq", [512, S], BF16)
    rk_d = scratch("rk", [512, S], BF16)
    rg_d = scratch("rg", [512, S], BF16)
    rkt_d = scratch("rkt", [S, 512], BF16)
    rv_d = scratch("rv", [S, 512], BF16)
    oT_d = scratch("oT", [2048, S], BF16)
    mods_d = scratch("mods", [L, 128, 48], F32) if "mods" in dbg else None

    def fm(ap):
        return ap.rearrange("(k p) t -> p k t", p=128)

    C = {}

    def load_const(k):
        shp, dt = CONST_SPECS[k]
        b = P.sb(shp, dt, "c_" + k)
        LD(b, b[:], cin[k][:])
        return b

    for k in ("ident_f", "ident_b", "ones_f", "blkones_f"):
        C[k] = load_const(k)
    vec = P.sb([128, L, NV], F32, "vec")
    LD(vec, vec[:], vecs_in.rearrange("l p v -> p l v"))
    mod = P.sb([128, L, 48], F32, "mod")
    A1 = P.sb([128, L, 8], F32, "A1")
    A2 = P.sb([128, L, 8], F32, "A2")
    lbv = P.sb([128, L, 4], F32, "lbv")
    oml = P.sb([128, L, 4], F32, "oml")

    def V(l, name, j=0, n=1):
        o = VOFF[name] + j
        return vec[:, l, o:o + n]

    with P.scope():
        csb = P.sb([128, KD], F32, "csb")
        sc2 = P.sb([128, KD, 2], F32, "sc2")
        LD(csb, csb[:], c_in[:])
        ACT(sc2[:, :, 0], csb[:], AF.Silu, r=[csb], w=[sc2])
        ACT(sc2[:, :, 1], csb[:], AF.Silu, r=[csb], w=[sc2])
        aw = [P.sb([128, KD, 1536], F32, f"aw{i}") for i in range(2)]
        mps = P.ps([128, 512], F32, "mps")
        it = 0
        for l in range(L):
            for g in range(4):
                a = aw[it % 2]
                it += 1
                for k in range(KD):
                    LD(a, a[:, k, :], ada_w[l, k * 128:(k + 1) * 128, g * 1536:(g + 1) * 1536])
                for jj in range(12):
                    j = g * 12 + jj
                    for k in range(KD):
                        MM(mps[:, 2 * j:2 * j + 2], a[:, k, jj * 128:(jj + 1) * 128], sc2[:, k, :],
                           start=(k == 0), stop=(k == KD - 1), r=[a, sc2], w=[mps])
            TT("dve", mod[:, l, :], mps[:, 0:96].rearrange("p (j two) -> p j two", two=2)[:, :, 0],
               V(l, "adab", 0, 48), ALU.add, r=[mps, vec], w=[mod])
            STT(A1[:, l, :], mod[:, l, 8:16], 1.0, V(l, "n1g", 0, 8), ALU.add, ALU.mult, r=[mod, vec], w=[A1])
            STT(A2[:, l, :], mod[:, l, 32:40], 1.0, V(l, "n2g", 0, 8), ALU.add, ALU.mult, r=[mod, vec], w=[A2])
            if mods_d is not None:
                ST(mods_d[l], mod, mod[:, l, :])
        ex = P.sb([128, L, 4], F32, "ex")
        ssum = P.sb([128, 4], F32, "ssum")
        ACT(ex[:], vec[:, :, VOFF["hlb"]:VOFF["hlb"] + 4], AF.Exp, r=[vec], w=[ex])
        CP("dve", ssum[:], ex[:, 0, :], r=[ex], w=[ssum])
        for l in range(1, L):
            TT("dve", ssum[:], ssum[:], ex[:, l, :], ALU.add, r=[ssum, ex], w=[ssum])
        OP("dve", lambda en: en.reciprocal(ssum[:], ssum[:]), r=[ssum], w=[ssum])
        OP("dve", lambda en: en.memset(lbv[:, 0, :], 0.0), w=[lbv])
        for l in range(1, L):
            TT("dve", ex[:, l, :], ex[:, l, :], ssum[:], ALU.mult, r=[ex, ssum], w=[ex])
            TT("dve", lbv[:, l, :], lbv[:, l - 1, :], ex[:, l, :], ALU.add, r=[lbv, ex], w=[lbv])
        TS("dve", oml[:], lbv[:], -1.0, 1.0, ALU.mult, ALU.add, r=[lbv], w=[oml])

    def phase_A(l, x_src):
        with P.scope():
            w = P.sb([128, KD, IN_COLS], BF16, "w_in")
            for k in range(KD):
                LD(w, w[:, k, :], w_in[l, k * 128:(k + 1) * 128, :], e="pool")
            xs = [P.sb([128, KD, 512], F32, f"xs{i}") for i in range(2)]
            hT = [P.sb([128, KD, 512], BF16, f"hT{i}") for i in range(2)]
            sqs = [P.sb([128, 512], F32, f"sq{i}") for i in range(2)]
            rstd = P.sb([128, 512], F32, "rstd")
            tmp = [P.sb([128, 512], F32, f"tmpa{i}") for i in range(2)]
            stg = [P.sb([128, 4, 512], BF16, f"stg{i}") for i in range(3)]
            stg32 = [P.sb([128, 4, 512], F32, f"stgf{i}") for i in range(1)]
            stt = [P.sb([128, 4, 512], BF16, f"stt{i}") for i in range(2)]
            pss = [P.ps([128, 512], F32, f"psa{i}") for i in range(7)]
            pstat = P.ps([128, 512], F32, "pstat")
            cnt = {"ps": 0, "stg": 0, "stt": 0, "ev": 0, "tmp": 0, "s32": 0}

            def nxt(key, lst):
                b = lst[cnt[key] % len(lst)]
                cnt[key] += 1
                return b

            for tb in range(cfg.NTB):
                tsl = slice(tb * 512, (tb + 1) * 512)
                x = xs[tb % 2]
                h = hT[tb % 2]
                LD(x, x[:], fm(x_src)[:, :, tsl])
                for k in range(KD):
                    sq = sqs[k % 2]
                    ACT(sq[:], x[:, k, :], AF.Square, r=[x], w=[sq])
                    MM(pstat[:], C["ones_f"][:], sq[:], start=(k == 0), stop=(k == KD - 1), r=[C["ones_f"], sq], w=[pstat])
                ACT(rstd[:], pstat[:], AF.Ln, bias=EPS, scale=1.0 / D, r=[pstat], w=[rstd])
                ACT(rstd[:], rstd[:], AF.Exp, scale=-0.5, r=[rstd], w=[rstd])
                for k in range(KD):
                    t_ = nxt("tmp", tmp)
                    TT("dve", t_[:], x[:, k, :], rstd[:], ALU.mult, r=[x, rstd], w=[t_])
                    ACT(h[:, k, :], t_[:], AF.Identity, bias=mod[:, l, k:k + 1], scale=A1[:, l, k:k + 1], r=[t_, mod, A1], w=[h])
                ST(fm(hT_d)[:, :, tsl], h, h[:])

                def proj_fm(c0):
                    p_ = nxt("ps", pss)
                    for k in range(KD):
                        MM(p_[:], w[:, k, c0:c0 + 128], h[:, k, :], start=(k == 0), stop=(k == KD - 1), r=[w, h], w=[p_])
                    return p_

                def simple_group(c0, dst, func):
                    s_ = nxt("stg", stg)
                    for j in range(4):
                        p_ = proj_fm(c0 + j * 128)
                        if func is None:
                            e = "act" if cnt["ev"] % 2 == 0 else "dve"
                            cnt["ev"] += 1
                            CP(e, s_[:, j, :], p_[:], r=[p_], w=[s_])
                        else:
                            ACT(s_[:, j, :], p_[:], func, r=[p_], w=[s_])
                    ST(fm(dst)[:, :, tsl], s_, s_[:])

                simple_group(0, hq_d, AF.Silu)
                simple_group(1536, hg_d, AF.Silu)
                s32 = nxt("s32", stg32)
                sk_ = nxt("stg", stg)
                for j in range(4):
                    p_ = proj_fm(512 + j * 128)
                    t_ = nxt("tmp", tmp)
                    ACT(t_[:], p_[:], AF.Sigmoid, r=[p_], w=[t_])
                    TS("dve", t_[:], t_[:], oml[:, l, j:j + 1], lbv[:, l, j:j + 1], ALU.mult, ALU.add, r=[t_, oml, lbv], w=[t_])
                    ACT(s32[:, j, :], t_[:], AF.Ln, r=[t_], w=[s32])
                    TS("dve", sk_[:, j, :], t_[:], -1.0, 1.0, ALU.mult, ALU.add, r=[t_], w=[sk_])
                ST(fm(hlf_d)[:, :, tsl], s32, s32[:])
                ST(fm(hk_d)[:, :, tsl], sk_, sk_[:])
                s_ = nxt("stg", stg)
                for j in range(4):
                    pa = proj_fm(2048 + j * 128)
                    pg = proj_fm(2560 + j * 128)
                    t_ = nxt("tmp", tmp)
                    ACT(t_[:], pg[:], AF.Sigmoid, r=[pg], w=[t_])
                    TT("dve", s_[:, j, :], pa[:], t_[:], ALU.mult, r=[pa, t_], w=[s_])
                ST(fm(cz_d)[:, :, tsl], s_, s_[:])
                simple_group(3072, sq_d, None)
                simple_group(3584, sk_d, None)
                simple_group(4608, rq_d, None)
                simple_group(5120, rk_d, None)
                simple_group(6144, rg_d, AF.Silu)
                for c0, dst in ((1024, hv_d), (4096, sv_d), (5120, rkt_d), (5632, rv_d)):
                    s_ = nxt("stt", stt)
                    for st in range(4):
                        p_ = nxt("ps", pss)
                        for k in range(KD):
                            MM(p_[:], h[:, k, st * 128:(st + 1) * 128], w[:, k, c0:c0 + 512], start=(k == 0), stop=(k == KD - 1), r=[w, h], w=[p_])
                        e = "act" if cnt["ev"] % 2 == 0 else "dve"
                        cnt["ev"] += 1
                        CP(e, s_[:, st, :], p_[:], r=[p_], w=[s_])
                    ST(dst[tsl, :].rearrange("(st p) c -> p st c", p=128), s_, s_[:])


    def phase_ret(l):
        with P.scope():
            cdec = load_const("ret_dec")
            cgp = load_const("ret_gp")
            ckd = load_const("ret_kd")
            rq = [P.sb([128, 4, 512], BF16, f"rq{i}") for i in range(2)]
            rk = [P.sb([128, 4, 512], BF16, f"rk{i}") for i in range(2)]
            rg = [P.sb([128, 4, 512], BF16, f"rg{i}") for i in range(2)]
            rkt = [P.sb([128, 4, 512], BF16, f"rkt{i}") for i in range(2)]
            rv = [P.sb([128, 4, 512], BF16, f"rv{i}") for i in range(2)]
            qd = P.sb([128, 4, 512], BF16, "qd")
            kdc = P.sb([128, 4, 512], BF16, "kdc")
            pt = [P.sb([128, 512], BF16, f"pt{h}") for h in range(4)]
            st32 = [P.sb([128, 128], F32, f"st32_{h}") for h in range(4)]
            stb = [[P.sb([128, 128], BF16, f"stb{h}_{i}") for i in range(2)] for h in range(4)]
            sqt = P.sb([128, 512], F32, "sqt")
            rs = P.sb([128, 512], F32, "rs")
            o1 = P.sb([128, 512], F32, "o1")
            osb = [P.sb([128, 4, 512], BF16, f"osb{i}") for i in range(2)]
            ps_o = [P.ps([128, 512], F32, f"pso{h}") for h in range(4)]
            ps_s = P.ps([128, 512], F32, "pss")
            ps_kv = [P.ps([128, 512], F32, f"pskv{i}") for i in range(2)]
            ps_ss = P.ps([128, 512], F32, "psss")
            gam128 = [float(np.exp(128.0 * np.log1p(-(2.0 ** (-5.0 - h))))) for h in range(4)]
            for h in range(4):
                OP("dve", lambda en, h=h: en.memset(st32[h][:], 0.0), w=[st32[h]])
            nkv = 0
            for tb in range(cfg.NTB):
                tsl = slice(tb * 512, (tb + 1) * 512)
                i = tb % 2
                LD(rq[i], rq[i][:], fm(rq_d)[:, :, tsl])
                LD(rk[i], rk[i][:], fm(rk_d)[:, :, tsl])
                LD(rg[i], rg[i][:], fm(rg_d)[:, :, tsl])
                LD(rkt[i], rkt[i][:], rkt_d[tsl, :].rearrange("(st p) c -> p st c", p=128))
                LD(rv[i], rv[i][:], rv_d[tsl, :].rearrange("(st p) c -> p st c", p=128))
                TT("dve", qd[:], rq[i][:], cgp[:], ALU.mult, r=[rq[i], cgp], w=[qd])
                for c in range(4):
                    TT("pool", kdc[:, c, :], rkt[i][:, c, :], ckd[:], ALU.mult, r=[rkt[i], ckd], w=[kdc])
                for h in range(4):
                    for c in range(4):
                        cs = slice(c * 128, (c + 1) * 128)
                        MM(ps_s[:, cs], rk[i][:, h, cs], rq[i][:, h, cs], r=[rk[i], rq[i]], w=[ps_s])
                    TT("dve", pt[h][:], ps_s[:], cdec[:, h, :], ALU.mult, r=[ps_s, cdec], w=[pt[h]])
                for c in range(4):
                    cs = slice(c * 128, (c + 1) * 128)
                    first = (tb == 0 and c == 0)
                    for h in range(4):
                        hs = slice(h * 128, (h + 1) * 128)
                        sb_ = stb[h][(tb * 4 + c) % 2]
                        MM(ps_o[h][:, cs], rv[i][:, c, hs], pt[h][:, cs], start=True, stop=first, r=[rv[i], pt[h]], w=[ps_o[h]])
                        if not first:
                            MM(ps_o[h][:, cs], sb_[:], qd[:, h, cs], start=False, stop=True, r=[sb_, qd], w=[ps_o[h]])
                        pk = ps_kv[nkv % 2]
                        nkv += 1
                        MM(pk[:, 0:128], kdc[:, c, hs], rv[i][:, c, hs], r=[kdc, rv[i]], w=[pk])
                        nb_ = stb[h][(tb * 4 + c + 1) % 2]
                        STT(nb_[:], st32[h][:], gam128[h], pk[:, 0:128], ALU.mult, ALU.add, r=[st32[h], pk], w=[nb_])
                        STT(st32[h][:], st32[h][:], gam128[h], pk[:, 0:128], ALU.mult, ALU.add, r=[st32[h], pk], w=[st32[h]])
                ob = osb[tb % 2]
                for h in range(4):
                    ACT(sqt[:], ps_o[h][:], AF.Square, r=[ps_o[h]], w=[sqt])
                    MM(ps_ss[:], C["ones_f"][:], sqt[:], r=[C["ones_f"], sqt], w=[ps_ss])
                    ACT(rs[:], ps_ss[:], AF.Ln, bias=EPS, scale=1.0 / 128, r=[ps_ss], w=[rs])
                    ACT(rs[:], rs[:], AF.Exp, scale=-0.5, r=[rs], w=[rs])
                    TT("dve", o1[:], ps_o[h][:], rs[:], ALU.mult, r=[ps_o[h], rs], w=[o1])
                    STT(ob[:, h, :], o1[:], V(l, "rtg", h), rg[i][:, h, :], ALU.mult, ALU.mult, r=[o1, vec, rg[i]], w=[ob])
                ST(fm(oT_d)[:, 12:16, tsl], ob, ob[:])

    def phase_conv(l):
        with P.scope():
            dg = P.sb([128, 124, 128], BF16, "dg")
            for kk in range(124):
                TS("dve" if kk % 2 == 0 else "pool", dg[:, kk, :], C["ident_b"][:], V(l, "cvw", kk), None, ALU.mult, r=[C["ident_b"], vec], w=[dg])
            z = [P.sb([128, 4, 542], BF16, f"z{i}") for i in range(2)]
            ysb = P.sb([128, 4, 512], F32, "ysb")
            ysq = [P.sb([128, 512], F32, f"ysq{i}") for i in range(2)]
            mean = P.sb([128, 512], F32, "mean")
            msq = P.sb([128, 512], F32, "msq")
            rs = P.sb([128, 512], F32, "rs")
            t1 = [P.sb([128, 512], F32, f"t1{i}") for i in range(2)]
            osb = [P.sb([128, 4, 512], BF16, f"osb{i}") for i in range(2)]
            pc = [P.ps([128, 512], F32, f"pc{i}") for i in range(2)]
            p1 = P.ps([128, 512], F32, "p1")
            p2 = P.ps([128, 512], F32, "p2")
            for tb in range(cfg.NTB):
                tsl = slice(tb * 512, (tb + 1) * 512)
                zz = z[tb % 2]
                if tb == 0:
                    OP("dve", lambda en, zz=zz: en.memset(zz[:, :, 0:30], 0.0), w=[zz])
                    LD(zz, zz[:, :, 30:542], fm(cz_d)[:, :, tsl])
                else:
                    LD(zz, zz[:], fm(cz_d)[:, :, tb * 512 - 30:(tb + 1) * 512])
                for ch in range(4):
                    p_ = pc[ch % 2]
                    for k in range(31):
                        MM(p_[:], dg[:, k * 4 + ch, :], zz[:, ch, k:k + 512], start=(k == 0), stop=(k == 30), r=[dg, zz], w=[p_])
                    ACT(ysb[:, ch, :], p_[:], AF.Identity, bias=V(l, "cvb", ch), r=[p_, vec], w=[ysb])
                    q_ = ysq[ch % 2]
                    ACT(q_[:], p_[:], AF.Square, bias=V(l, "cvb", ch), r=[p_, vec], w=[q_])
                    MM(p1[:], C["ones_f"][:], ysb[:, ch, :], start=(ch == 0), stop=(ch == 3), r=[C["ones_f"], ysb], w=[p1])
                    MM(p2[:], C["ones_f"][:], q_[:], start=(ch == 0), stop=(ch == 3), r=[C["ones_f"], q_], w=[p2])
                ACT(mean[:], p1[:], AF.Identity, scale=1.0 / 512, r=[p1], w=[mean])
                TT("dve", msq[:], mean[:], mean[:], ALU.mult, r=[mean], w=[msq])
                STT(rs[:], p2[:], 1.0 / 512, msq[:], ALU.mult, ALU.subtract, r=[p2, msq], w=[rs])
                ACT(rs[:], rs[:], AF.Ln, bias=EPS, r=[rs], w=[rs])
                ACT(rs[:], rs[:], AF.Exp, scale=-0.5, r=[rs], w=[rs])
                ob = osb[tb % 2]
                for ch in range(4):
                    t_ = t1[ch % 2]
                    TT("dve", t_[:], ysb[:, ch, :], mean[:], ALU.subtract, r=[ysb, mean], w=[t_])
                    TT("dve", t_[:], t_[:], rs[:], ALU.mult, r=[t_, rs], w=[t_])
                    ACT(ob[:, ch, :], t_[:], AF.Silu, bias=V(l, "lnb", ch), scale=V(l, "lng", ch), r=[t_, vec], w=[ob])
                ST(fm(oT_d)[:, 4:8, tsl], ob, ob[:])

    def phase_hgrn(l):
        SBK = min(1024, S)
        NTT = SBK // 128
        NCH = SBK // 32
        NHD = 4
        with P.scope():
            cmask = load_const("hg_mask")
            cscan = load_const("hg_scanmask")
            lf = P.sb([128, SBK], F32, "lf")
            qh = P.sb([128, SBK], BF16, "qh")
            kh = P.sb([128, SBK], BF16, "kh")
            b = P.sb([128, SBK], F32, "b")
            e1 = P.sb([128, SBK], F32, "e1")
            e2 = P.sb([128, SBK], F32, "e2")
            ks = P.sb([128, SBK], BF16, "ks")
            gh = [P.sb([128, SBK], BF16, f"gh{h}") for h in range(NHD)]
            vh = [P.sb([128, NTT, 128], BF16, f"vh{h}") for h in range(NHD)]
            vh32 = [P.sb([32, NCH, 128], BF16, f"vh32{h}") for h in range(NHD)]
            qd = [P.sb([128, SBK], BF16, f"qd{h}") for h in range(NHD)]
            qd32 = [P.sb([128, SBK], F32, f"qd32{h}") for h in range(NHD)]
            kt = [P.sb([128, SBK], BF16, f"kt{h}") for h in range(NHD)]
            kst = [P.sb([32, NCH, 128], BF16, f"kst{h}") for h in range(NHD)]
            dcc = P.sb([128, NCH, NHD], F32, "dcc")
            sdec = P.sb([128, NHD, 128], F32, "sdec")
            stb = [P.sb([128, NHD, 128], F32, f"stb{i}") for i in range(2)]
            ptb = [P.sb([128, NHD, 128], BF16, f"ptb{i}") for i in range(2)]
            sqt = P.sb([128, 512], F32, "sqt")
            rs = P.sb([128, 512], F32, "rs")
            o1 = P.sb([128, 512], F32, "o1")
            osb = [P.sb([128, 512], BF16, f"osb{i}") for i in range(2)]
            po = [P.ps([128, 512], F32, f"pso{h}") for h in range(NHD)]
            ps_t = P.ps([128, 512], BF16, "pst")
            ps_s = P.ps([128, 512], F32, "pss")
            ps_kv = P.ps([128, 512], F32, "pskv")
            ps_ss = P.ps([128, 512], F32, "psss")
            no = 0
            nst = 0
            OP("dve", lambda en: en.memset(stb[0][:], 0.0), w=[stb[0]])
            for sbk in range(S // SBK):
                csl = slice(sbk * SBK, (sbk + 1) * SBK)
                for h in range(NHD):
                    hs = slice(h * 128, (h + 1) * 128)
                    LD(lf, lf[:], hlf_d[hs, csl])
                    LD(qh, qh[:], hq_d[hs, csl])
                    LD(kh, kh[:], hk_d[hs, csl])
                    LD(gh[h], gh[h][:], hg_d[hs, csl])
                    LD(vh[h], vh[h][:], hv_d[csl, hs].rearrange("(tt p) c -> p tt c", p=128))
                    LD(vh32[h], vh32[h][:], hv_d[csl, hs].rearrange("(cc p) c -> p cc c", p=32))
                    OP("dve", lambda en: en.tensor_tensor_scan(b[:], cscan[:, 0:SBK], lf[:], 0.0, ALU.mult, ALU.add), r=[cscan, lf], w=[b])
                    TS("dve", b[:], b[:], -75.0, None, ALU.max, r=[b], w=[b])
                    ACT(e1[:], b[:], AF.Exp, r=[b], w=[e1])
                    ACT(e2[:], b[:], AF.Exp, scale=-1.0, r=[b], w=[e2])
                    TT("dve", qd32[h][:], qh[:], e1[:], ALU.mult, r=[qh, e1], w=[qd32[h]])
                    CP("pool", qd[h][:], qd32[h][:], r=[qd32[h]], w=[qd[h]])
                    TT("pool", kt[h][:], kh[:], e2[:], ALU.mult, r=[kh, e2], w=[kt[h]])
                    b3 = b[:].rearrange("p (c j) -> p c j", j=32)
                    ACT(dcc[:, :, h], b3[:, :, 31], AF.Exp, r=[b], w=[dcc])
                    TT("dve", e1[:].rearrange("p (c j) -> p c j", j=32), b3[:, :, 31:32].to_broadcast([128, NCH, 32]), b3,
                       ALU.subtract, r=[b, e1], w=[e1])
                    ACT(e1[:], e1[:], AF.Exp, r=[e1], w=[e1])
                    TT("pool", ks[:], kh[:], e1[:], ALU.mult, r=[kh, e1], w=[ks])
                    for c4 in range(NCH // 4):
                        for j in range(4):
                            ci = c4 * 4 + j
                            TR(ps_t[0:32, j * 128:(j + 1) * 128], ks[:, ci * 32:(ci + 1) * 32], C["ident_b"][:], r=[ks, C["ident_b"]], w=[ps_t])
                        CP("act", kst[h][:, c4 * 4:(c4 + 1) * 4, :], ps_t[0:32, :].rearrange("p (j c) -> p j c", c=128), r=[ps_t], w=[kst[h]])
                for t4 in range(NTT // 4):
                    for j in range(4):
                        tt = t4 * 4 + j
                        tl = slice(tt * 128, (tt + 1) * 128)
                        js = slice(j * 128, (j + 1) * 128)
                        pb = ptb[tt % 2]
                        for h in range(NHD):
                            MM(ps_s[:, h * 128:(h + 1) * 128], kt[h][:, tl], qd[h][:, tl], r=[kt[h], qd[h]], w=[ps_s])
                        TT("dve", pb[:].rearrange("p h t -> p (h t)"), ps_s[:], cmask[:], ALU.mult, r=[ps_s, cmask], w=[pb])
                        for h in range(NHD):
                            MM(po[h][:, js], vh[h][:, tt, :], pb[:, h, :], start=True, stop=False, r=[vh[h], pb], w=[po[h]])
                        for c in range(4):
                            ci = tt * 4 + c
                            cur = stb[nst % 2]
                            nx_ = stb[(nst + 1) % 2]
                            nst += 1
                            for h in range(NHD):
                                MM(po[h][:, j * 128 + c * 32:j * 128 + (c + 1) * 32], cur[:, h, :], qd32[h][:, tt * 128 + c * 32:tt * 128 + (c + 1) * 32],
                                   start=False, stop=(c == 3), r=[cur, qd32[h]], w=[po[h]])
                                MM(ps_kv[:, h * 128:(h + 1) * 128], kst[h][:, ci, :], vh32[h][:, ci, :], r=[kst[h], vh32[h]], w=[ps_kv])
                            TT("dve", sdec[:], cur[:], dcc[:, ci, :].unsqueeze(2).to_broadcast([128, NHD, 128]), ALU.mult, r=[cur, dcc], w=[sdec])
                            kv3 = ps_kv[:].rearrange("p (h v) -> p h v", v=128)
                            TT("dve", nx_[:], sdec[:], kv3, ALU.add, r=[sdec, ps_kv], w=[nx_])
                    c5 = slice(t4 * 512, (t4 + 1) * 512)
                    for h in range(NHD):
                        ACT(sqt[:], po[h][:], AF.Square, r=[po[h]], w=[sqt])
                        MM(ps_ss[:], C["ones_f"][:], sqt[:], r=[C["ones_f"], sqt], w=[ps_ss])
                        ACT(rs[:], ps_ss[:], AF.Ln, bias=EPS, scale=1.0 / 128, r=[ps_ss], w=[rs])
                        ACT(rs[:], rs[:], AF.Exp, scale=-0.5, r=[rs], w=[rs])
                        TT("dve", o1[:], po[h][:], rs[:], ALU.mult, r=[po[h], rs], w=[o1])
                        ob = osb[no % 2]
                        no += 1
                        STT(ob[:], o1[:], V(l, "hgng", h), gh[h][:, c5], ALU.mult, ALU.mult, r=[o1, vec, gh[h]], w=[ob])
                        ST(oT_d[h * 128:(h + 1) * 128, sbk * SBK + t4 * 512:sbk * SBK + (t4 + 1) * 512], ob, ob[:])

    def phase_sb(l):
        NT = cfg.NT
        QW = 1024
        NH = QW // 512
        NJ = QW // 128
        with P.scope():
            ctri = load_const("sb_trineg")
            cones = load_const("sb_onesneg")
            cdm = load_const("sb_dmask")
            g8 = P.sb([128, 2], F32, "g8")
            TS("dve", g8[:, 0:1], V(l, "qng"), 0.125, None, ALU.mult, r=[vec], w=[g8])
            CP("dve", g8[:, 1:2], V(l, "kng"), r=[vec], w=[g8])
            qraw = P.sb([128, S], BF16, "qraw")
            qn = P.sb([128, S], BF16, "qn")
            kn = P.sb([128, S], BF16, "kn")
            vv = P.sb([128, NT, 128], BF16, "vv")
            sqt = [P.sb([128, 512], F32, f"sqt{i}") for i in range(2)]
            rs = [P.sb([128, 512], F32, f"rs{i}") for i in range(2)]
            et = [P.sb([128, QW], F32, f"et{i}") for i in range(2)]
            spb = [P.sb([128, QW], BF16, f"spb{i}") for i in range(3)]
            wb = [P.sb([128, QW], BF16, f"wb{i}") for i in range(3)]
            accb = [P.sb([128, QW], BF16, f"acc{i}") for i in range(3)]
            osb = [P.sb([64, QW], BF16, f"osb{i}") for i in range(2)]
            ps_z = [P.ps([128, QW], F32, f"psz{i}") for i in range(2)]
            ps_c = P.ps([128, QW], F32, "psc")
            ps_o = P.ps([128, QW], F32, "pso")
            n = {"pair": 0, "o": 0}
            for hp in range(4):
                rows = slice(hp * 128, (hp + 1) * 128)
                LD(vv, vv[:], sv_d[:, rows].rearrange("(tt p) c -> p tt c", p=128))
                for src, dst, gi in ((sq_d, qn, 0), (sk_d, kn, 1)):
                    LD(qraw, qraw[:], src[rows, :])
                    for pc_ in range(S // 512):
                        cs = slice(pc_ * 512, (pc_ + 1) * 512)
                        q_ = sqt[pc_ % 2]
                        r_ = rs[pc_ % 2]
                        ACT(q_[:], qraw[:, cs], AF.Square, r=[qraw], w=[q_])
                        MM(ps_z[0][:, 0:512], C["blkones_f"][:], q_[:], r=[C["blkones_f"], q_], w=[ps_z[0]])
                        ACT(r_[:], ps_z[0][:, 0:512], AF.Ln, bias=EPS, scale=1.0 / 64, r=[ps_z[0]], w=[r_])
                        ACT(r_[:], r_[:], AF.Exp, scale=-0.5, r=[r_], w=[r_])
                        STT(dst[:, cs], qraw[:, cs], g8[:, gi:gi + 1], r_[:], ALU.mult, ALU.mult, r=[qraw, g8, r_], w=[dst])
                pairs = []
                for a in range(2):
                    for qt in range(S // QW):
                        kbs = list(range(NJ * qt + NJ - 1, -1, -1))
                        for idx, kb in enumerate(kbs):
                            pairs.append(dict(a=a, qt=qt, kb=kb, idx=idx, last=(idx == len(kbs) - 1), j=kb - NJ * qt))
                NP = len(pairs)

                def S1(p):
                    d = pairs[p]
                    po_ = slice(d["a"] * 64, (d["a"] + 1) * 64)
                    ks_ = slice(d["kb"] * 128, (d["kb"] + 1) * 128)
                    pz = ps_z[p % 2]
                    e_ = et[p % 2]
                    sp = spb[p % 3]
                    for hh in range(NH):
                        hs_ = slice(hh * 512, (hh + 1) * 512)
                        qs = slice(d["qt"] * QW + hh * 512, d["qt"] * QW + (hh + 1) * 512)
                        MM(pz[:, hs_], kn[po_, ks_], qn[po_, qs], r=[kn, qn], w=[pz])
                    ACT(e_[:], pz[:], AF.Exp, r=[pz], w=[e_])
                    ACT(sp[:], e_[:], AF.Ln, bias=1.0, r=[e_], w=[sp])
                    if d["j"] >= 0:
                        TT("pool", sp[:], sp[:], cdm[:, d["j"], :], ALU.mult, r=[sp, cdm], w=[sp])
                    if not d["last"]:
                        nacc = accb[(p + 1) % 3]
                        if d["idx"] == 0:
                            CP("dve", nacc[:], sp[:], r=[sp], w=[nacc])
                        else:
                            TT("dve", nacc[:], accb[p % 3][:], sp[:], ALU.add, r=[accb[p % 3], sp], w=[nacc])

                def S2(p):
                    d = pairs[p]
                    po_ = slice(d["a"] * 64, (d["a"] + 1) * 64)
                    ks_ = slice(d["kb"] * 128, (d["kb"] + 1) * 128)
                    sp = spb[p % 3]
                    w_ = wb[p % 3]
                    acc = None if d["idx"] == 0 else accb[p % 3]
                    for hh in range(NH):
                        hs_ = slice(hh * 512, (hh + 1) * 512)
                        qs = slice(d["qt"] * QW + hh * 512, d["qt"] * QW + (hh + 1) * 512)
                        MM(ps_c[:, hs_], kn[po_, ks_], qn[po_, qs], start=True, stop=False, r=[kn, qn], w=[ps_c])
                        MM(ps_c[:, hs_], ctri[:], sp[:, hs_], start=False, stop=(acc is None), r=[ctri, sp], w=[ps_c])
                        if acc is not None:
                            MM(ps_c[:, hs_], cones[:], acc[:, hs_], start=False, stop=True, r=[cones, acc], w=[ps_c])
                    ACT(w_[:], ps_c[:], AF.Exp, r=[ps_c], w=[w_])
                    if d["j"] >= 0:
                        TT("pool", w_[:], w_[:], cdm[:, d["j"], :], ALU.mult, r=[w_, cdm], w=[w_])

                def S3(p):
                    d = pairs[p]
                    po_ = slice(d["a"] * 64, (d["a"] + 1) * 64)
                    w_ = wb[p % 3]
                    for hh in range(NH):
                        hs_ = slice(hh * 512, (hh + 1) * 512)
                        MM(ps_o[0:64, hs_], vv[:, d["kb"], po_], w_[:, hs_], start=(d["idx"] == 0), stop=d["last"], r=[vv, w_], w=[ps_o])
                    if d["last"]:
                        ob = osb[n["o"] % 2]
                        n["o"] += 1
                        CP("dve", ob[:], ps_o[0:64, :], r=[ps_o], w=[ob])
                        r0 = 1024 + hp * 128 + d["a"] * 64
                        ST(oT_d[r0:r0 + 64, d["qt"] * QW:(d["qt"] + 1) * QW], ob, ob[:])

                for t in range(NP + 2):
                    if t < NP:
                        S1(t)
                    if 0 <= t - 1 < NP:
                        S2(t - 1)
                    if 0 <= t - 2 < NP:
                        S3(t - 2)

    def phase_C(l, x_src, x_dst):
        with P.scope():
            wg = P.sb([128, KD, 4096], BF16, "wg")
            wbr = P.sb([128, 16, 1024], BF16, "wbr")
            wo = P.sb([128, KD, 1024], BF16, "wo")
            for k in range(KD):
                LD(wg, wg[:, k, :], w_gate[l, k * 128:(k + 1) * 128, :], e="pool")
            for r_ in range(16):
                LD(wbr, wbr[:, r_, :], w_branch[l, r_ * 128:(r_ + 1) * 128, :], e="pool")
            for k in range(KD):
                LD(wo, wo[:, k, :], w_out[l, k * 128:(k + 1) * 128, :], e="pool")
            hT = [P.sb([128, KD, 512], BF16, f"hT{i}") for i in range(2)]
            oT = [P.sb([128, 16, 512], BF16, f"oT{i}") for i in range(2)]
            xs = P.sb([128, KD, 512], F32, "xs")
            gs = [P.sb([128, 512], F32, f"gs{i}") for i in range(2)]
            tmp = [P.sb([128, 512], F32, f"tmp{i}") for i in range(2)]
            m32 = P.sb([128, 512], F32, "m32")
            mg = P.sb([128, KD, 512], BF16, "mg")
            pg = [P.ps([128, 512], F32, f"pg{i}") for i in range(2)]
            py = [P.ps([128, 512], F32, f"py{i}") for i in range(2)]
            po = [P.ps([128, 512], F32, f"po{i}") for i in range(2)]

            def loads(tb):
                tsl = slice(tb * 512, (tb + 1) * 512)
                LD(hT[tb % 2], hT[tb % 2][:], fm(hT_d)[:, :, tsl])
                LD(oT[tb % 2], oT[tb % 2][:], fm(oT_d)[:, :, tsl])

            loads(0)
            it = 0
            for tb in range(cfg.NTB):
                tsl = slice(tb * 512, (tb + 1) * 512)
                if tb + 1 < cfg.NTB:
                    loads(tb + 1)
                LD(xs, xs[:], fm(x_src)[:, :, tsl])
                h = hT[tb % 2]
                o = oT[tb % 2]
                for dc in range(KD):
                    for n in range(4):
                        g_ = pg[it % 2]
                        y_ = py[it % 2]
                        s_ = gs[it % 2]
                        t_ = tmp[it % 2]
                        it += 1
                        c0 = n * 1024 + dc * 128
                        for k in range(KD):
                            MM(g_[:], wg[:, k, c0:c0 + 128], h[:, k, :], start=(k == 0), stop=(k == KD - 1), r=[wg, h], w=[g_])
                        for r_ in range(4):
                            MM(y_[:], wbr[:, n * 4 + r_, dc * 128:(dc + 1) * 128], o[:, n * 4 + r_, :], start=(r_ == 0), stop=(r_ == 3), r=[wbr, o], w=[y_])
                        ACT(s_[:], g_[:], AF.Sigmoid, bias=V(l, "bgate", n * 8 + dc), r=[g_, vec], w=[s_])
                        if n == 0:
                            TT("dve", m32[:], y_[:], s_[:], ALU.mult, r=[y_, s_], w=[m32])
                        else:
                            TT("dve", t_[:], y_[:], s_[:], ALU.mult, r=[y_, s_], w=[t_])
                            if n < 3:
                                TT("pool", m32[:], m32[:], t_[:], ALU.add, r=[m32, t_], w=[m32])
                            else:
                                TT("pool", mg[:, dc, :], m32[:], t_[:], ALU.add, r=[m32, t_], w=[mg])
                for dc in range(KD):
                    p_ = po[dc % 2]
                    t_ = tmp[dc % 2]
                    for k in range(KD):
                        MM(p_[:], wo[:, k, dc * 128:(dc + 1) * 128], mg[:, k, :], start=(k == 0), stop=(k == KD - 1), r=[wo, mg], w=[p_])
                    ACT(t_[:], p_[:], AF.Identity, scale=mod[:, l, 16 + dc:17 + dc], r=[p_, mod], w=[t_])
                    TT("dve", xs[:, dc, :], xs[:, dc, :], t_[:], ALU.add, r=[xs, t_], w=[xs])
                ST(fm(x_dst)[:, :, tsl], xs, xs[:])

    gdbg_d = scratch("gdbg", [S, 32], F32) if "gdbg" in dbg else None
    TBK = min(2048, S)
    NTI = TBK // 128
    NSB = TBK // 512

    def phase_D(l, x_src, x_dst):
        for blk in range(S // TBK):
            t0 = blk * TBK
            with P.scope():
                h2T = P.sb([128, KD, TBK], BF16, "h2T")
                G = P.sb([128, NTI, 32], F32, "G")
                acc = P.sb([128, NTI, 1024], F32, "acc")
                with P.scope():
                    rw = P.sb([128, KD, 36], F32, "rw")
                    rb = P.sb([128, 36], F32, "rb")
                    LD(rw, rw[:], rw_in[l].rearrange("(k p) c -> p k c", p=128))
                    LD(rb, rb[:], rb_in[l])
                    xs = [P.sb([128, KD, 512], F32, f"xs{i}") for i in range(2)]
                    sqs = [P.sb([128, 512], F32, f"sq{i}") for i in range(2)]
                    rstd = P.sb([128, 512], F32, "rstd")
                    tmp = [P.sb([128, 512], F32, f"tmp{i}") for i in range(2)]
                    h2f = P.sb([128, KD, 512], F32, "h2f")
                    lg = P.sb([128, 4, 36], F32, "lg")
                    sm = {n_: P.sb([128, 4], F32, "r_" + n_) for n_ in ("m4", "s4", "gp", "m1", "m2", "r", "w1", "w2")}
                    e4 = P.sb([128, 4, 4], F32, "e4")
                    oh4 = P.sb([128, 4, 4], F32, "oh4")
                    t48 = P.sb([128, 4, 4, 8], F32, "t48")
                    sel8 = P.sb([128, 4, 8], F32, "sel8")
                    sel8b = P.sb([128, 4, 8], F32, "sel8b")
                    oh1 = P.sb([128, 4, 8], F32, "oh1")
                    oh2 = P.sb([128, 4, 8], F32, "oh2")
                    g8 = P.sb([128, 4, 8], F32, "g8")
                    pstat = P.ps([128, 512], F32, "pstat")
                    ps_r = P.ps([128, 512], F32, "psr")

                    def bc(ap, shape):
                        return ap.to_broadcast(shape)

                    def RED(out, in_, op, r, w):
                        OP("dve", lambda en: en.tensor_reduce(out, in_, AX.X, op), r=r, w=w)

                    LD(xs[0], xs[0][:], fm(x_src)[:, :, t0:t0 + 512])
                    for sbi in range(NSB):
                        x = xs[sbi % 2]
                        if sbi + 1 < NSB:
                            LD(xs[(sbi + 1) % 2], xs[(sbi + 1) % 2][:], fm(x_src)[:, :, t0 + (sbi + 1) * 512:t0 + (sbi + 2) * 512])
                        for k in range(KD):
                            sq = sqs[k % 2]
                            ACT(sq[:], x[:, k, :], AF.Square, r=[x], w=[sq])
                            MM(pstat[:], C["ones_f"][:], sq[:], start=(k == 0), stop=(k == KD - 1), r=[C["ones_f"], sq], w=[pstat])
                        ACT(rstd[:], pstat[:], AF.Ln, bias=EPS, scale=1.0 / D, r=[pstat], w=[rstd])
                        ACT(rstd[:], rstd[:], AF.Exp, scale=-0.5, r=[rstd], w=[rstd])
                        for k in range(KD):
                            t_ = tmp[k % 2]
                            TT("dve", t_[:], x[:, k, :], rstd[:], ALU.mult, r=[x, rstd], w=[t_])
                            ACT(h2f[:, k, :], t_[:], AF.Identity, bias=mod[:, l, 24 + k:25 + k], scale=A2[:, l, k:k + 1], r=[t_, mod, A2], w=[h2f])
                            CP("pool", h2T[:, k, sbi * 512:(sbi + 1) * 512], h2f[:, k, :], r=[h2f], w=[h2T])
                        for i in range(4):
                            for k in range(KD):
                                MM(ps_r[:, i * 36:(i + 1) * 36], h2f[:, k, i * 128:(i + 1) * 128], rw[:, k, :], start=(k == 0), stop=(k == KD - 1), r=[h2f, rw], w=[ps_r])
                        TT("dve", lg[:], ps_r[:, 0:144].rearrange("p (i c) -> p i c", c=36), rb[:].unsqueeze(1).to_broadcast([128, 4, 36]), ALU.add, r=[ps_r, rb], w=[lg])
                        lgg = lg[:, :, 0:4]
                        el = lg[:, :, 4:36].rearrange("p i (g e) -> p i g e", e=8)
                        RED(sm["m4"][:], lgg, ALU.max, [lg], [sm["m4"]])
                        TT("dve", oh4[:], lgg, bc(sm["m4"][:].unsqueeze(2), [128, 4, 4]), ALU.is_equal, r=[lg, sm["m4"]], w=[oh4])
                        TT("dve", e4[:], lgg, bc(sm["m4"][:].unsqueeze(2), [128, 4, 4]), ALU.subtract, r=[lg, sm["m4"]], w=[e4])
                        ACT(e4[:], e4[:], AF.Exp, r=[e4], w=[e4])
                        RED(sm["s4"][:], e4[:], ALU.add, [e4], [sm["s4"]])
                        OP("dve", lambda en: en.reciprocal(sm["gp"][:], sm["s4"][:]), r=[sm["s4"]], w=[sm["gp"]])
                        TT("dve", t48[:], el, bc(oh4[:].unsqueeze(3), [128, 4, 4, 8]), ALU.mult, r=[lg, oh4], w=[t48])
                        RED(sel8[:], t48[:].rearrange("p i g e -> p i e g"), ALU.add, [t48], [sel8])
                        RED(sm["m1"][:], sel8[:], ALU.max, [sel8], [sm["m1"]])
                        TT("dve", oh1[:], sel8[:], bc(sm["m1"][:].unsqueeze(2), [128, 4, 8]), ALU.is_equal, r=[sel8, sm["m1"]], w=[oh1])
                        STT(sel8b[:], oh1[:], -1e30, sel8[:], ALU.mult, ALU.add, r=[oh1, sel8], w=[sel8b])
                        RED(sm["m2"][:], sel8b[:], ALU.max, [sel8b], [sm["m2"]])
                        TT("dve", oh2[:], sel8b[:], bc(sm["m2"][:].unsqueeze(2), [128, 4, 8]), ALU.is_equal, r=[sel8b, sm["m2"]], w=[oh2])
                        TT("dve", sm["r"][:], sm["m2"][:], sm["m1"][:], ALU.subtract, r=[sm["m2"], sm["m1"]], w=[sm["r"]])
                        ACT(sm["r"][:], sm["r"][:], AF.Exp, r=[sm["r"]], w=[sm["r"]])
                        TS("dve", sm["w1"][:], sm["r"][:], 1.0, None, ALU.add, r=[sm["r"]], w=[sm["w1"]])
                        OP("dve", lambda en: en.reciprocal(sm["w1"][:], sm["w1"][:]), r=[sm["w1"]], w=[sm["w1"]])
                        TT("dve", sm["w2"][:], sm["w1"][:], sm["r"][:], ALU.mult, r=[sm["w1"], sm["r"]], w=[sm["w2"]])
                        TT("dve", sm["w1"][:], sm["w1"][:], sm["gp"][:], ALU.mult, r=[sm["w1"], sm["gp"]], w=[sm["w1"]])
                        TT("dve", sm["w2"][:], sm["w2"][:], sm["gp"][:], ALU.mult, r=[sm["w2"], sm["gp"]], w=[sm["w2"]])
                        TT("dve", oh1[:], oh1[:], bc(sm["w1"][:].unsqueeze(2), [128, 4, 8]), ALU.mult, r=[oh1, sm["w1"]], w=[oh1])
                        TT("dve", oh2[:], oh2[:], bc(sm["w2"][:].unsqueeze(2), [128, 4, 8]), ALU.mult, r=[oh2, sm["w2"]], w=[oh2])
                        TT("dve", g8[:], oh1[:], oh2[:], ALU.add, r=[oh1, oh2], w=[g8])
                        TT("dve", G[:, sbi * 4:(sbi + 1) * 4, :].rearrange("p i (g e) -> p i g e", e=8), bc(oh4[:].unsqueeze(3), [128, 4, 4, 8]),
                           bc(g8[:].unsqueeze(2), [128, 4, 4, 8]), ALU.mult, r=[oh4, g8], w=[G])
                    if gdbg_d is not None:
                        ST(gdbg_d[t0:t0 + TBK, :].rearrange("(i p) c -> p i c", p=128), G, G[:])
                if getattr(cfg, "d_stage", 9) < 2:
                    continue
                with P.scope():
                    w1 = [P.sb([128, KD, 512], BF16, f"w1_{i}") for i in range(2)]
                    w3 = [P.sb([128, KD, 512], BF16, f"w3_{i}") for i in range(2)]
                    w2 = [P.sb([128, 4, 1024], BF16, f"w2_{i}") for i in range(2)]
                    aT = P.sb([128, 4, TBK], BF16, "aT")
                    sl = [P.sb([128, 512], F32, f"sl{i}") for i in range(2)]
                    yt = [P.sb([128, 1024], F32, f"yt{i}") for i in range(2)]
                    ph1 = [P.ps([128, 512], F32, f"ph1{i}") for i in range(2)]
                    ph3 = [P.ps([128, 512], F32, f"ph3{i}") for i in range(2)]
                    py = [P.ps([128, 1024], F32, f"py{i}") for i in range(2)]

                    def wloads(e):
                        i = e % 2
                        LD(w1[i], w1[i][:], ew1[l, e].rearrange("(k p) f -> p k f", p=128), e="pool")
                        LD(w3[i], w3[i][:], ew3[l, e].rearrange("(k p) f -> p k f", p=128), e="pool")
                        LD(w2[i], w2[i][:], ew2[l, e].rearrange("(k p) d -> p k d", p=128), e="pool")

                    wloads(0)
                    it = 0
                    iy = 0
                    for e in range(N_EXP):
                        i = e % 2
                        if e + 1 < N_EXP:
                            wloads(e + 1)
                        for sbi in range(NSB):
                            cs = slice(sbi * 512, (sbi + 1) * 512)
                            for fc in range(4):
                                a_ = ph1[it % 2]
                                b_ = ph3[it % 2]
                                s_ = sl[it % 2]
                                it += 1
                                for k in range(KD):
                                    MM(a_[:], w1[i][:, k, fc * 128:(fc + 1) * 128], h2T[:, k, cs], start=(k == 0), stop=(k == KD - 1), r=[w1[i], h2T], w=[a_])
                                for k in range(KD):
                                    MM(b_[:], w3[i][:, k, fc * 128:(fc + 1) * 128], h2T[:, k, cs], start=(k == 0), stop=(k == KD - 1), r=[w3[i], h2T], w=[b_])
                                ACT(s_[:], a_[:], AF.Silu, r=[a_], w=[s_])
                                TT("dve", aT[:, fc, cs], b_[:], s_[:], ALU.mult, r=[b_, s_], w=[aT])
                        for tt in range(NTI):
                            p_ = py[iy % 2]
                            y_ = yt[iy % 2]
                            iy += 1
                            for dh in range(2):
                                for fc in range(4):
                                    MM(p_[:, dh * 512:(dh + 1) * 512], aT[:, fc, tt * 128:(tt + 1) * 128], w2[i][:, fc, dh * 512:(dh + 1) * 512],
                                       start=(fc == 0), stop=(fc == 3), r=[aT, w2[i]], w=[p_])
                            if e == 0:
                                ACT(acc[:, tt, :], p_[:], AF.Identity, scale=G[:, tt, e:e + 1], r=[p_, G], w=[acc])
                            else:
                                ACT(y_[:], p_[:], AF.Identity, scale=G[:, tt, e:e + 1], r=[p_, G], w=[y_])
                                TT("dve", acc[:, tt, :], acc[:, tt, :], y_[:], ALU.add, r=[acc, y_], w=[acc])
                if getattr(cfg, "d_stage", 9) < 3:
                    continue
                with P.scope():
                    xs = [P.sb([128, KD, 512], F32, f"xo{i}") for i in range(2)]
                    tmp = [P.sb([128, 512], F32, f"tmpo{i}") for i in range(2)]
                    pt = [P.ps([128, 512], F32, f"pto{i}") for i in range(2)]
                    n_ = 0
                    for sbi in range(NSB):
                        x = xs[sbi % 2]
                        LD(x, x[:], fm(x_src)[:, :, t0 + sbi * 512:t0 + (sbi + 1) * 512])
                        for k in range(KD):
                            p_ = pt[n_ % 2]
                            t_ = tmp[n_ % 2]
                            n_ += 1
                            for j in range(4):
                                MM(p_[:, j * 128:(j + 1) * 128], acc[:, sbi * 4 + j, k * 128:(k + 1) * 128], C["ident_f"][:], r=[acc, C["ident_f"]], w=[p_])
                            ACT(t_[:], p_[:], AF.Identity, scale=mod[:, l, 40 + k:41 + k], r=[p_, mod], w=[t_])
                            TT("dve", x[:, k, :], x[:, k, :], t_[:], ALU.add, r=[x, t_], w=[x])
                        ST(fm(x_dst)[:, :, t0 + sbi * 512:t0 + (sbi + 1) * 512], x, x[:])

    return nc, P, dict(phase_A=phase_A, phase_ret=phase_ret, phase_conv=phase_conv, phase_hgrn=phase_hgrn, phase_sb=phase_sb,
                       phase_C=phase_C, phase_D=phase_D,
                       xT_in=xT_in, xA=xA, xB=xB, out_T=out_T)


def _chunkcols(v):
    return np.ascontiguousarray(np.asarray(v, np.float32).reshape(-1, 128).T)


def make_in_maps(inp, cfg):
    L = cfg.L
    B = inp["x"].shape[0]
    vecs = np.zeros((L, 128, NV), np.float32)
    for l in range(L):
        def put(name, arr):
            a = _chunkcols(arr)
            vecs[l][:, VOFF[name]:VOFF[name] + a.shape[1]] = a
        put("n1g", inp["norm1_g"][l]); put("n2g", inp["norm2_g"][l]); put("adab", inp["ada_b"][l])
        put("hgng", inp["hgrn_norm_g"][l]); put("cvb", inp["conv_b"][l]); put("lng", inp["conv_ln_g"][l])
        put("lnb", inp["conv_ln_b"][l]); put("rtg", inp["ret_norm_g"][l]); put("bgate", inp["b_gate"][l])
        put("hlb", inp["hgrn_lb"][l])
        vecs[l][:, VOFF["qng"]] = np.tile(np.asarray(inp["sb_qnorm_g"][l], np.float32), 2)
        vecs[l][:, VOFF["kng"]] = np.tile(np.asarray(inp["sb_knorm_g"][l], np.float32), 2)
        cw = np.asarray(inp["conv_w"][l], np.float32)
        for k in range(31):
            vecs[l][:, VOFF["cvw"] + 4 * k:VOFF["cvw"] + 4 * k + 4] = _chunkcols(cw[k])
    rw = np.ascontiguousarray(np.concatenate([np.asarray(inp["router_group_w"], np.float32)[:L],
                                              np.asarray(inp["router_expert_w"], np.float32)[:L]], axis=-1))
    rb1 = np.concatenate([np.asarray(inp["router_group_b"], np.float32)[:L], np.asarray(inp["router_expert_b"], np.float32)[:L]], axis=-1)
    rb = np.ascontiguousarray(np.broadcast_to(rb1[:, None, :], (L, 128, 36)))
    consts = host_consts()
    shared = {
        "vecs": vecs, "rw": rw, "rb": rb,
        "ada_w": np.ascontiguousarray(np.asarray(inp["ada_w"], np.float32)[:L]),
        "w_in": np.ascontiguousarray(np.asarray(inp["w_in"], np.float32)[:L]),
        "w_branch": np.ascontiguousarray(np.asarray(inp["w_branch"], np.float32)[:L]),
        "w_gate": np.ascontiguousarray(np.asarray(inp["w_gate"], np.float32)[:L]),
        "w_out": np.ascontiguousarray(np.asarray(inp["w_out"], np.float32)[:L]),
        "ew1": np.ascontiguousarray(np.asarray(inp["expert_w1"], np.float32)[:L]),
        "ew3": np.ascontiguousarray(np.asarray(inp["expert_w3"], np.float32)[:L]),
        "ew2": np.ascontiguousarray(np.asarray(inp["expert_w2"], np.float32)[:L]),
    }
    for k, v in consts.items():
        shared["k_" + k] = v
    maps = []
    for b in range(B):
        m = dict(shared)
        m["xT"] = np.ascontiguousarray(np.asarray(inp["x"][b], np.float32).T)
        m["c"] = _chunkcols(inp["c"][b])
        maps.append(m)
    return maps


def build_full(cfg):
    nc, P, ph = build_program(cfg)
    L = cfg.L
    src = ph["xT_in"]
    for l in range(L):
        ph["phase_A"](l, src)
        ph["phase_conv"](l)
        ph["phase_ret"](l)
        ph["phase_hgrn"](l)
        ph["phase_sb"](l)
        ph["phase_C"](l, src, ph["xA"])
        dst = ph["out_T"] if l == L - 1 else ph["xB"]
        ph["phase_D"](l, ph["xA"], dst)
        src = dst
    P.barrier()
    P.emit()
    return nc, P


def kernel(**inputs):
    x = np.asarray(inputs["x"])
    B, S, _ = x.shape
    L = np.asarray(inputs["w_in"]).shape[0]
    cfg = Cfg(S=S, L=L)
    nc, P = build_full(cfg)
    maps = make_in_maps(inputs, cfg)
    res = run_bass_kernel_spmd(nc, maps, core_ids=list(range(len(maps))))
    out = np.stack([np.ascontiguousarray(np.asarray(r["outT"]).T) for r in res.results], axis=0)
    return out.astype(np.float32)
```
